# Optimizing a Trainium2 kernel written in Bass

```python
import jax, jax.numpy as jnp
from jax import lax
import numpy as np

D_MODEL = 1024
BATCH = 16
SEQ = 2048
DEPTH = 1

RET_HEADS = 4
RET_QK_DIM = D_MODEL // RET_HEADS
RET_V_DIM = 2 * RET_QK_DIM
RET_QK = RET_HEADS * RET_QK_DIM
RET_V = RET_HEADS * RET_V_DIM
CHUNK = 128
ROPE_BASE = 10000.0
CONV_CH = D_MODEL
CONV_WIDTH = 31
D_FF = 2816
EPS = 1e-6
N_SUB = 3
MAX_POS_OFFSET = 4096
IN_SPLITS = [RET_QK, RET_QK, RET_V, RET_V, CONV_CH, CONV_CH, D_MODEL, D_MODEL]
IN_COLS = sum(IN_SPLITS)

kernel_name = "hybrid_retention_conformer_macaron_adaln"


def rms_norm(x, g):
    xf = x.astype(jnp.float32)
    y = xf * lax.rsqrt(jnp.mean(xf * xf, axis=-1, keepdims=True) + EPS)
    return (y * g.astype(jnp.float32)).astype(x.dtype)


def layer_norm(x, g, b):
    xf = x.astype(jnp.float32)
    mu = jnp.mean(xf, axis=-1, keepdims=True)
    var = jnp.mean(jnp.square(xf - mu), axis=-1, keepdims=True)
    y = (xf - mu) * lax.rsqrt(var + EPS)
    return (y * g.astype(jnp.float32) + b.astype(jnp.float32)).astype(x.dtype)


def modulate(xn, shift, scale):
    return xn * (1.0 + scale[:, None, :]) + shift[:, None, :]


def swiglu_ffn(x, w_gu, w_d):
    gate, up = jnp.split(x @ w_gu, 2, axis=-1)
    return (jax.nn.silu(gate) * up) @ w_d


def rope(x, positions):
    d = x.shape[-1]
    half = d // 2
    freqs = jnp.power(ROPE_BASE, -jnp.arange(0, half, dtype=jnp.float32) * (2.0 / d))
    ang = positions.astype(jnp.float32)[..., None] * freqs
    cos = jnp.cos(ang)[:, :, None, :]
    sin = jnp.sin(ang)[:, :, None, :]
    x1, x2 = x[..., :half], x[..., half:]
    return jnp.concatenate([x1 * cos - x2 * sin, x1 * sin + x2 * cos], axis=-1)


def retention_chunkwise(q, k, v):
    B, S, H, dk = q.shape
    dv = v.shape[-1]
    n_chunks = S // CHUNK
    log_gamma = jnp.log1p(-jnp.power(2.0, -5.0 - jnp.arange(H, dtype=jnp.float32)))
    idx = jnp.arange(CHUNK, dtype=jnp.float32)
    diff = idx[:, None] - idx[None, :]
    intra = jnp.where(diff[None] >= 0,
                      jnp.exp(log_gamma[:, None, None] * jnp.maximum(diff, 0.0)[None]), 0.0)
    xi = jnp.exp(log_gamma[:, None] * (idx + 1.0))[None, :, :, None]
    zeta = jnp.exp(log_gamma[:, None] * (CHUNK - 1.0 - idx))[None, :, :, None]
    gamma_c = jnp.exp(log_gamma * CHUNK)[None, :, None, None]

    def to_chunks(t):
        d = t.shape[-1]
        return t.reshape(B, n_chunks, CHUNK, H, d).transpose(1, 0, 3, 2, 4)

    def step(state, xs):
        qc, kc, vc = xs
        s = jnp.einsum('bhqd,bhkd->bhqk', qc, kc) * intra
        o = (jnp.einsum('bhqk,bhkv->bhqv', s, vc)
             + jnp.einsum('bhqd,bhdv->bhqv', qc, state) * xi)
        state = state * gamma_c + jnp.einsum('bhkd,bhkv->bhdv', kc * zeta, vc)
        return state, o

    state0 = jnp.zeros((B, H, dk, dv), jnp.float32)
    _, o = lax.scan(step, state0, (to_chunks(q), to_chunks(k), to_chunks(v)))
    return o.transpose(1, 0, 3, 2, 4).reshape(B, S, H, dv)


def head_group_norm(o, g, b):
    mu = jnp.mean(o, axis=-1, keepdims=True)
    var = jnp.mean(jnp.square(o - mu), axis=-1, keepdims=True)
    y = ((o - mu) * lax.rsqrt(var + EPS)).reshape(o.shape[0], o.shape[1], -1)
    return y * g.astype(jnp.float32) + b.astype(jnp.float32)


def hybrid_mixer(xn, positions, w_in, ret_gn_g, ret_gn_b, w_ret_o, w_dw, b_dw,
                 conv_ln_g, conv_ln_b, w_conv_o, b_conv_o, w_out):
    B, S, _ = xn.shape
    z = xn @ w_in
    cuts = list(np.cumsum(IN_SPLITS)[:-1])
    q, k, v, g_ret, glu_a, glu_b, gate_ret, gate_conv = jnp.split(z, cuts, axis=-1)

    qf = rope(q.astype(jnp.float32).reshape(B, S, RET_HEADS, RET_QK_DIM), positions)
    kf = rope(k.astype(jnp.float32).reshape(B, S, RET_HEADS, RET_QK_DIM), positions) * (RET_QK_DIM ** -0.5)
    vf = v.astype(jnp.float32).reshape(B, S, RET_HEADS, RET_V_DIM)
    o = head_group_norm(retention_chunkwise(qf, kf, vf), ret_gn_g, ret_gn_b).astype(xn.dtype)
    y_ret = (jax.nn.silu(g_ret) * o) @ w_ret_o

    u = glu_a * jax.nn.sigmoid(glu_b)
    u = lax.conv_general_dilated(u, w_dw.astype(u.dtype), window_strides=(1,),
                                 padding=[(CONV_WIDTH - 1, 0)],
                                 dimension_numbers=('NWC', 'WIO', 'NWC'),
                                 feature_group_count=CONV_CH) + b_dw
    u = jax.nn.silu(layer_norm(u, conv_ln_g, conv_ln_b))
    y_conv = u @ w_conv_o + b_conv_o

    merged = jax.nn.sigmoid(gate_ret) * y_ret + jax.nn.sigmoid(gate_conv) * y_conv
    return merged @ w_out


def setup_inputs(seed: int = 0) -> dict:
    key = jax.random.key(seed)
    ks = iter(jax.random.split(key, 32))
    f32 = jnp.float32

    def w(shape, fan_in, scale=1.0):
        return jax.random.normal(next(ks), shape, f32) * (scale * fan_in ** -0.5)

    def gain(shape):
        return 1.0 + 0.05 * jax.random.normal(next(ks), shape, f32)

    def bias(shape):
        return 0.02 * jax.random.normal(next(ks), shape, f32)

    L = DEPTH
    x = jax.random.normal(next(ks), (BATCH, SEQ, D_MODEL), f32)
    c = jax.random.normal(next(ks), (BATCH, D_MODEL), f32)
    offset = jax.random.randint(next(ks), (BATCH, 1), 0, MAX_POS_OFFSET, dtype=jnp.int32)
    positions = offset + jnp.arange(SEQ, dtype=jnp.int32)[None, :]
    return {
        "x": x,
        "c": c,
        "positions": positions,
        "w_mod": w((L, D_MODEL, 3 * N_SUB * D_MODEL), D_MODEL),
        "b_mod": bias((L, 3 * N_SUB * D_MODEL)),
        "g_norm1": gain((L, D_MODEL)),
        "w_ffn1_gu": w((L, D_MODEL, 2 * D_FF), D_MODEL),
        "w_ffn1_d": w((L, D_FF, D_MODEL), D_FF),
        "g_norm2": gain((L, D_MODEL)),
        "w_in": w((L, D_MODEL, IN_COLS), D_MODEL),
        "ret_gn_g": gain((L, RET_V)),
        "ret_gn_b": bias((L, RET_V)),
        "w_ret_o": w((L, RET_V, D_MODEL), RET_V),
        "w_dw": w((L, CONV_WIDTH, 1, CONV_CH), CONV_WIDTH),
        "b_dw": bias((L, CONV_CH)),
        "conv_ln_g": gain((L, CONV_CH)),
        "conv_ln_b": bias((L, CONV_CH)),
        "w_conv_o": w((L, CONV_CH, D_MODEL), CONV_CH),
        "b_conv_o": bias((L, D_MODEL)),
        "w_out": w((L, D_MODEL, D_MODEL), D_MODEL),
        "g_norm3": gain((L, D_MODEL)),
        "w_ffn2_gu": w((L, D_MODEL, 2 * D_FF), D_MODEL),
        "w_ffn2_d": w((L, D_FF, D_MODEL), D_FF),
        "g_normf": gain((D_MODEL,)),
    }


def reference(x, c, positions, w_mod, b_mod, g_norm1, w_ffn1_gu, w_ffn1_d, g_norm2,
              w_in, ret_gn_g, ret_gn_b, w_ret_o, w_dw, b_dw, conv_ln_g, conv_ln_b,
              w_conv_o, b_conv_o, w_out, g_norm3, w_ffn2_gu, w_ffn2_d, g_normf):
    h = x
    c_act = jax.nn.silu(c)
    for l in range(DEPTH):
        mod = c_act @ w_mod[l] + b_mod[l]
        (sh1, sc1, gt1, sh2, sc2, gt2, sh3, sc3, gt3) = jnp.split(mod, 3 * N_SUB, axis=-1)
        xn = modulate(rms_norm(h, g_norm1[l]), sh1, sc1)
        h = h + 0.5 * gt1[:, None, :] * swiglu_ffn(xn, w_ffn1_gu[l], w_ffn1_d[l])
        xn = modulate(rms_norm(h, g_norm2[l]), sh2, sc2)
        h = h + gt2[:, None, :] * hybrid_mixer(xn, positions, w_in[l], ret_gn_g[l], ret_gn_b[l],
                                               w_ret_o[l], w_dw[l], b_dw[l], conv_ln_g[l],
                                               conv_ln_b[l], w_conv_o[l], b_conv_o[l], w_out[l])
        xn = modulate(rms_norm(h, g_norm3[l]), sh3, sc3)
        h = h + 0.5 * gt3[:, None, :] * swiglu_ffn(xn, w_ffn2_gu[l], w_ffn2_d[l])
    return rms_norm(h, g_normf)
```

```python
import math
from contextlib import ExitStack

import numpy as np
import concourse.bass as bass
import concourse.mybir as mybir
from concourse.bass_utils import run_bass_kernel_spmd

F32 = mybir.dt.float32
BF16 = mybir.dt.bfloat16
I32 = mybir.dt.int32
AF = mybir.ActivationFunctionType
ALU = mybir.AluOpType

NCORE = 8
D = 1024
SEQ = 2048
T = 512
NTILE = SEQ // T
SPC = 2
TOK = SPC * SEQ
DFF = 2816
HC = DFF // 128
EPS = 1e-6
NS = 4
SLOT_E = 4096
NHEAD = 4
PST_N = 2
SAME_ENG_WINDOW = 3
CW = 31

V_GN = 0
V_BMOD = 32
V_RGG = 104
V_RGB = 120
V_BDW = 136
V_LNG = 144
V_LNB = 152
V_BCO = 160
V_WDW = 168
V_FREQ = 416
V_ZETA = 417
V_XI = 421
NV = 425

GAMMA = [1.0 - 2.0 ** (-5.0 - h) for h in range(NHEAD)]
RC1 = 6.28125
RC2 = 2.0 * math.pi - RC1
PI_LO = 3.1415925


class Reg:
    __slots__ = ("w", "rs")

    def __init__(self):
        self.w = None
        self.rs = {}


def regs(n):
    return [Reg() for _ in range(n)]


class Sched:
    ENGS = ("pe", "act", "dve", "pool", "sp")

    def __init__(self):
        self.q = {e: [] for e in self.ENGS}
        self.cnt = {}
        self.waited = {e: {} for e in self.ENGS}

    def op(self, eng, fn, reads=(), writes=(), dma_sem=None):
        deps = {}

        def add(k, v):
            if deps.get(k, 0) < v:
                deps[k] = v

        for r in reads:
            if r.w is not None:
                add(*r.w)
        for w in writes:
            if w.w is not None:
                if not (w.w[0] == eng and dma_sem is None):
                    add(*w.w)
            for k, v in w.rs.items():
                if k == eng and dma_sem is None:
                    continue
                add(k, v)
        waits = []
        for k, v in deps.items():
            if k == eng and (eng == "pe" or v <= self.cnt.get(eng, 0) - SAME_ENG_WINDOW):
                continue
            if self.waited[eng].get(k, 0) < v:
                self.waited[eng][k] = v
                waits.append((k, v))
        key = eng if dma_sem is None else dma_sem
        inc = 1 if dma_sem is None else 16
        self.cnt[key] = self.cnt.get(key, 0) + inc
        val = self.cnt[key]
        self.q[eng].append((waits, fn, key, inc))
        for r in reads:
            if r.rs.get(key, 0) < val:
                r.rs[key] = val
        for w in writes:
            w.w = (key, val)
            w.rs = {}
        return (key, val)

    def final_wait(self, eng, key):
        self.q[eng].append(([(key, self.cnt[key])], None, None, 0))

    def emit(self, eng, e, sems):
        for waits, fn, key, inc in self.q[eng]:
            for k, v in waits:
                e.wait_ge(sems[k], v)
            if fn is not None:
                ins = fn(e)
                ins.then_inc(sems[key], inc)


def build_program(n_seq=SPC, n_tile=NTILE, stop_after="final", do_mod=True, mix_stop=99):
    nc = bass.Bass("TRN2", target_bir_lowering=False)
    S = Sched()

    def din(name, shape, dt=F32):
        return nc.dram_tensor(name, shape, dt, kind="ExternalInput").ap()

    xT = din("xT", [128, 8, TOK])
    posrep = din("posrep", [128, TOK], I32)
    cT = din("cT", [128, 16])
    vecs_d = din("vecs", [128, NV])
    mask_d = din("maskT", [128, NHEAD * 128])
    ident_d = din("ident", [128, 128])
    wmod = din("wmod", [18, 128, 4096])
    wgu = [din("wgu1", [11, 128, 4096]), din("wgu2", [11, 128, 4096])]
    wd = [din("wd1", [8, 128, 2816]), din("wd2", [8, 128, 2816])]
    win = din("win", [20, 128, 4096])
    wro = din("wro", [4, 128, 4096])
    wco = din("wco", [2, 128, 4096])
    wout = din("wout", [2, 128, 4096])
    outT = nc.dram_tensor("outT", [128, 8, TOK], F32, kind="ExternalOutput").ap()

    with ExitStack() as es:
        def sb(name, shape, dt):
            return es.enter_context(nc.sbuf_tensor(name, shape, dt))

        def pst(name, shape, dt):
            return es.enter_context(nc.psum_tensor(name, shape, dt))

        h = sb("h", [128, 8, T], F32)
        xn = sb("xn", [128, 8, T], BF16)
        hid = sb("hid", [128, HC, T], BF16)
        slots = [sb(f"slot{i}", [128, SLOT_E], BF16) for i in range(NS)]
        qf = sb("qf", [128, 8, T], BF16)
        kf = sb("kf", [128, 8, T], BF16)
        kT2 = sb("kT", [128, 4 * 1024], BF16)
        kT = kT2[:, :].rearrange("p (a b) -> p a b", a=4)
        t1 = kT2[:, :].rearrange("p (a b) -> p a b", a=8)
        vT = sb("vT", [128, 4, 2048], BF16)
        u = sb("u", [128, 8, 30 + T], BF16)
        gr = sb("gr", [128, 8, T], BF16)
        gc = sb("gc", [128, 8, T], BF16)
        stf = sb("stf", [128, 8, 512], F32)
        stb = sb("stb", [128, 8, 512], BF16)
        cosT = sb("cosT", [128, T], F32)
        sinT = sb("sinT", [128, T], F32)
        posi = sb("posi", [128, T], I32)
        sqb = sb("sqb", [128, 4, T], BF16)
        tmpf = sb("tmpf", [128, 4, T], F32)
        tmpb = sb("tmpb", [128, 4, T], BF16)
        rstd = sb("rstd", [128, 2, T], F32)
        onrm = sb("onrm", [128, 4, 512], BF16)
        Ssb = sb("Ssb", [128, 2, 128], BF16)
        accf = sb("accf", [128, 2, T], F32)
        vecs = sb("vecs_s", [128, NV], F32)
        maskT = sb("mask_s", [128, NHEAD * 128], F32)
        ident = sb("ident_s", [128, 128], BF16)
        ones = sb("ones_s", [128, 128], BF16)
        cTs = sb("cTs", [128, 16], F32)
        cact = sb("cact", [128, 16], BF16)
        modT = sb("modT", [128, 2 * 72], F32)
        g32 = sb("g32", [128, 32], F32)
        drv = sb("drv", [128, 2 * 3 * 2 * 8], F32)
        bnst = sb("bnst", [128, 2, 6], F32)
        bnmv = sb("bnmv", [128, 2, 2], F32)
        dg = sb("dg", [128, 4, 128], BF16)
        bnr = sb("bnr", [128, 2, 1], F32)
        bnv = sb("bnv", [128, 2, 1], F32)
        bna = sb("bna", [128, 2, 1], F32)

        NB = 7
        banks = [pst(f"pb{i}", [128, 512], F32) for i in range(NB)]

        rh, rxn, rhid = regs(8), regs(8), regs(HC)
        rslot = regs(NS)
        rq, rk, rkT = regs(8), regs(8), regs(8)
        rvT = [regs(4) for _ in range(4)]
        ru, rgr, rgc = regs(8), regs(8), regs(8)
        rstf, rstb = regs(8), regs(8)
        rcos, rsin, rposi = Reg(), Reg(), Reg()
        rsq, rtmpf, rtmpb = regs(4), regs(4), regs(4)
        rrstd, ronrm, rSsb, raccf = regs(2), regs(4), regs(2), regs(2)
        rbank, rpsT = regs(NB), regs(2)
        rvecs, rmask, rident, rones, rcT, rcact, rmod, rg32, rdrv = (Reg() for _ in range(9))
        rbn = regs(2)
        rdg = regs(4)
        rlnmean = Reg()

        rot = {"bank": 0, "sq": 0, "tmpf": 0, "tmpb": 0, "rstd": 0, "Ssb": 0, "accf": 0, "psT": 0,
               "slot": 0, "bn": 0, "dg": 0}

        def nxt(name, n):
            i = rot[name]
            rot[name] = (i + 1) % n
            return i

        def bank():
            i = nxt("bank", NB)
            return banks[i], rbank[i]

        def get_tmpf():
            i = nxt("tmpf", 4)
            return tmpf[:, i, :], rtmpf[i]

        def get_tmpb():
            i = nxt("tmpb", 4)
            return tmpb[:, i, :], rtmpb[i]

        def mm(out_ap, pairs, reads, writes, start=True, stop=True):
            def fn(e, out_ap=out_ap, pairs=pairs, start=start, stop=stop):
                n = len(pairs)
                ins = None
                for i, (l, r) in enumerate(pairs):
                    ins = e.matmul(out_ap, lhsT=l, rhs=r, start=(start and i == 0), stop=(stop and i == n - 1))
                return ins
            S.op("pe", fn, reads, writes)

        def transposes(items, reads, writes):
            def fn(e, items=items):
                ins = None
                for o, i_ in items:
                    ins = e.matmul(o, lhsT=i_, rhs=ident[:, :], start=True, stop=True)
                return ins
            S.op("pe", fn, reads + [rident], writes)

        def act(out, in_, func, reads, writes, bias=None, scale=None):
            def fn(e, out=out, in_=in_, func=func, bias=bias, scale=scale):
                kw = {}
                if bias is not None:
                    kw["bias"] = bias
                if scale is not None:
                    kw["scale"] = scale
                return e.activation(out=out, in_=in_, func=func, **kw)
            S.op("act", fn, reads, writes)

        def dve_tt(out, in0, in1, op, reads, writes):
            S.op("dve", lambda e, out=out, in0=in0, in1=in1, op=op: e.tensor_tensor(out=out, in0=in0, in1=in1, op=op),
                 reads, writes)

        def dve_ts(out, in0, s1, s2, op0, op1, reads, writes):
            def fn(e, out=out, in0=in0, s1=s1, s2=s2, op0=op0, op1=op1):
                if op1 is None:
                    return e.tensor_scalar(out=out, in0=in0, scalar1=s1, scalar2=None, op0=op0)
                return e.tensor_scalar(out=out, in0=in0, scalar1=s1, scalar2=s2, op0=op0, op1=op1)
            S.op("dve", fn, reads, writes)

        def dve_stt(out, in0, scalar, in1, op0, op1, reads, writes):
            S.op("dve", lambda e, out=out, in0=in0, scalar=scalar, in1=in1, op0=op0, op1=op1:
                 e.scalar_tensor_tensor(out=out, in0=in0, scalar=scalar, in1=in1, op0=op0, op1=op1), reads, writes)

        def dve_copy(out, in_, reads, writes):
            S.op("dve", lambda e, out=out, in_=in_: e.tensor_copy(out=out, in_=in_), reads, writes)

        def dma(eng, out, in_, reads, writes, semkey):
            S.op(eng, lambda e, out=out, in_=in_: e.dma_start(out=out, in_=in_), reads, writes, dma_sem=semkey)

        def next_piece(dram_piece, E):
            i = nxt("slot", NS)
            dma("pool", slots[i][:, 0:E], dram_piece, [], [rslot[i]], f"slot{i}")
            return slots[i], rslot[i]

        def vcol(base, i=0, n=1):
            return vecs[:, base + i: base + i + n]

        dma("sp", vecs[:, :], vecs_d[:, :], [], [rvecs], "const")
        dma("sp", maskT[:, :], mask_d[:, :], [], [rmask], "const")
        dma("sp", cTs[:, :], cT[:, :], [], [rcT], "const")
        dma("pool", ident[:, :], ident_d[:, :], [], [rident], "identld")
        S.op("dve", lambda e: e.memset(ones[:, :], 1.0), [], [rones])
        act(cact[:, :], cTs[:, :], AF.Silu, [rcT], [rcact])
        dve_ts(g32[:, :], vecs[:, V_GN:V_GN + 32], 1.0, None, ALU.mult, None, [rvecs], [rg32])
        dve_ts(vecs[:, V_WDW:V_WDW + 8 * CW], vecs[:, V_WDW:V_WDW + 8 * CW], 0.5, None, ALU.mult, None, [rvecs], [rvecs])
        for p in range(18 if do_mod else 0):
            sl, rsl = next_piece(wmod[p, :, :], 4096)
            for cc in range(4):
                ch = 4 * p + cc
                pb, rpb = bank()
                mm(pb[:, 0:2], [(sl[:, kc * 512 + cc * 128: kc * 512 + cc * 128 + 128], cact[:, kc * 2: kc * 2 + 2])
                                for kc in range(8)], [rsl, rcact], [rpb])
                for s in range(2):
                    dve_ts(modT[:, s * 72 + ch: s * 72 + ch + 1], pb[:, s:s + 1], vcol(V_BMOD, ch), None, ALU.add, None,
                           [rpb, rvecs], [rmod])

        def drvA(s, i):
            o = ((s * 3 + i) * 2 + 0) * 8
            return drv[:, o:o + 8]

        def drvG(s, i):
            o = ((s * 3 + i) * 2 + 1) * 8
            return drv[:, o:o + 8]

        def modB(s, i):
            o = s * 72 + (3 * i) * 8
            return modT[:, o:o + 8]

        for s in range(2):
            for i in range(3):
                sc = modT[:, s * 72 + (3 * i + 1) * 8: s * 72 + (3 * i + 1) * 8 + 8]
                gt = modT[:, s * 72 + (3 * i + 2) * 8: s * 72 + (3 * i + 2) * 8 + 8]
                dve_stt(drvA(s, i), sc, 1.0, g32[:, 8 * i: 8 * i + 8], ALU.add, ALU.mult, [rmod, rg32], [rdrv])
                dve_ts(drvG(s, i), gt, 0.5, None, ALU.mult, None, [rmod], [rdrv])

        MAGIC = 0x5f3759df

        def rsqrt_newton(y, ry, v, rv, a, ra):
            yi = y.bitcast(I32)
            dve_ts(yi, v.bitcast(I32), 1, None, ALU.arith_shift_right, None, [rv], [ry])
            dve_ts(yi, yi, -1, MAGIC, ALU.mult, ALU.add, [ry], [ry])
            for _ in range(2):
                dve_stt(a, y, -0.5, y, ALU.mult, ALU.mult, [ry], [ra])
                dve_tt(a, a, v, ALU.mult, [ra, rv], [ra])
                dve_stt(y, a, 1.5, y, ALU.add, ALU.mult, [ra, ry], [ry])

        def sumsq_rstd():
            pb, rpb = bank()
            for c in range(8):
                i = nxt("sq", 4)
                act(sqb[:, i, :], h[:, c, :], AF.Square, [rh[c]], [rsq[i]])
                mm(pb[:, :], [(ones[:, :], sqb[:, i, :])], [rsq[i], rones], [rpb], start=(c == 0), stop=(c == 7))
            j = nxt("rstd", 2)
            vv, rvv = get_tmpf()
            aa, raa = get_tmpf()
            dve_ts(vv, pb[:, :], 1.0 / D, EPS, ALU.mult, ALU.add, [rpb], [rvv])
            rsqrt_newton(rstd[:, j, :], rrstd[j], vv, rvv, aa, raa)
            return rstd[:, j, :], rrstd[j]

        def norm_mod(s, i):
            rs_ap, rrs = sumsq_rstd()
            A = drvA(s, i)
            Bv = modB(s, i)
            for c in range(8):
                tf, rtf = get_tmpf()
                dve_tt(tf, h[:, c, :], rs_ap, ALU.mult, [rh[c], rrs], [rtf])
                act(xn[:, c, :], tf, AF.Identity, [rtf, rdrv, rmod], [rxn[c]], bias=Bv[:, c:c + 1], scale=A[:, c:c + 1])

        def ffn(s, i, which):
            G = drvG(s, i)
            for p in range(11):
                sl, rsl = next_piece(wgu[which][p, :, :], 4096)
                for jj in range(2):
                    j = 2 * p + jj
                    gb, rgb = bank()
                    ub, rub = bank()
                    mm(gb[:, :], [(sl[:, kc * 512 + jj * 256: kc * 512 + jj * 256 + 128], xn[:, kc, :]) for kc in range(8)],
                       [rsl] + rxn, [rgb])
                    mm(ub[:, :], [(sl[:, kc * 512 + jj * 256 + 128: kc * 512 + jj * 256 + 256], xn[:, kc, :]) for kc in range(8)],
                       [rsl] + rxn, [rub])
                    tb, rtb = get_tmpb()
                    act(tb, gb[:, :], AF.Silu, [rgb], [rtb])
                    dve_tt(hid[:, j, :], ub[:, :], tb, ALU.mult, [rub, rtb], [rhid[j]])
            for m in range(8):
                sl, rsl = next_piece(wd[which][m, :, :], 2816)
                yb, ryb = bank()
                mm(yb[:, :], [(sl[:, kc * 128: kc * 128 + 128], hid[:, kc, :]) for kc in range(HC)], [rsl] + rhid, [ryb])
                dve_stt(h[:, m, :], yb[:, :], G[:, m:m + 1], h[:, m, :], ALU.mult, ALU.add, [ryb, rdrv, rh[m]], [rh[m]])

        def rope(x1, r1, x2, r2, o1, ro1, o2, ro2):
            ta, rta = get_tmpf()
            tb_, rtb_ = get_tmpf()
            dve_tt(ta, x1, cosT[:, :], ALU.mult, [r1, rcos], [rta])
            dve_tt(tb_, x2, sinT[:, :], ALU.mult, [r2, rsin], [rtb_])
            dve_tt(o1, ta, tb_, ALU.subtract, [rta, rtb_], [ro1])
            tc_, rtc_ = get_tmpf()
            td, rtd = get_tmpf()
            dve_tt(tc_, x1, sinT[:, :], ALU.mult, [r1, rsin], [rtc_])
            dve_tt(td, x2, cosT[:, :], ALU.mult, [r2, rcos], [rtd])
            dve_tt(o2, tc_, td, ALU.add, [rtc_, rtd], [ro2])

        def fm_group(sl, rsl, off):
            pb, rpb = bank()
            mm(pb[:, :], [(sl[:, kc * 512 + off: kc * 512 + off + 128], xn[:, kc, :]) for kc in range(8)], [rsl] + rxn, [rpb])
            return pb, rpb

        def mixer(s, first_tile):
            norm_mod(s, 1)
            pi = 0
            for which, dst, rdst in ((0, qf, rq), (1, kf, rk)):
                for pp in range(2):
                    sl, rsl = next_piece(win[pi, :, :], 4096)
                    pi += 1
                    for hh in range(2):
                        hd = 2 * pp + hh
                        p1, rp1 = fm_group(sl, rsl, (2 * hh) * 128)
                        p2, rp2 = fm_group(sl, rsl, (2 * hh + 1) * 128)
                        rope(p1[:, :], rp1, p2[:, :], rp2, dst[:, 2 * hd, :], rdst[2 * hd], dst[:, 2 * hd + 1, :], rdst[2 * hd + 1])
            if mix_stop <= 1:
                return
            for c in range(8):
                hd = c // 2
                tb_k, rtb_k = bank()
                transposes([(tb_k[:, n * 128:(n + 1) * 128], kf[:, c, n * 128:(n + 1) * 128]) for n in range(4)], [rk[c]], [rtb_k])
                act(kT[:, :, c * 128:(c + 1) * 128], tb_k[:, :].rearrange("p (a b) -> p a b", a=4), AF.Identity, [rtb_k, rvecs],
                    [rkT[c]], scale=vcol(V_ZETA, hd))
            if mix_stop <= 2:
                return
            for hd in range(4):
                sl, rsl = next_piece(win[pi, :, :], 4096)
                pi += 1
                for n in range(4):
                    pb, rpb = bank()
                    import os
                    if os.environ.get("DBG_V", "both") in ("both", "mm"):
                        mm(pb[:, :], [(xn[:, kc, n * 128:(n + 1) * 128], sl[:, kc * 512: kc * 512 + 512]) for kc in range(8)],
                           [rsl] + rxn, [rpb])
                    if os.environ.get("DBG_V", "both") in ("both", "act"):
                        act(vT[:, n, hd * 512:(hd + 1) * 512], pb[:, :], AF.Identity, [rpb], [rvT[n][hd]])
            if mix_stop <= 3:
                return
            for pp in range(4):
                sl, rsl = next_piece(win[pi, :, :], 4096)
                pi += 1
                for cc in range(4):
                    j = 4 * pp + cc
                    pb, rpb = fm_group(sl, rsl, cc * 128)
                    act(hid[:, j, :], pb[:, :], AF.Silu, [rpb], [rhid[j]])
            if mix_stop <= 4:
                return
            if first_tile:
                for c in range(8):
                    S.op("dve", lambda e, c=c: e.memset(u[:, c, 0:30], 0.0), [], [ru[c]])
            for pp in range(4):
                sl, rsl = next_piece(win[pi, :, :], 4096)
                pi += 1
                for e_ in range(2):
                    c = 2 * pp + e_
                    pa, rpa = fm_group(sl, rsl, e_ * 128)
                    pb, rpb = fm_group(sl, rsl, (2 + e_) * 128)
                    tb, rtb = get_tmpb()
                    act(tb, pb[:, :], AF.Tanh, [rpb], [rtb], scale=0.5)
                    dve_stt(u[:, c, 30:30 + T], tb, 1.0, pa[:, :], ALU.add, ALU.mult, [rpa, rtb], [ru[c]])
            if mix_stop <= 5:
                return
            for dst, rdst in ((gr, rgr), (gc, rgc)):
                for pp in range(2):
                    sl, rsl = next_piece(win[pi, :, :], 4096)
                    pi += 1
                    for cc in range(4):
                        c = 4 * pp + cc
                        pb, rpb = fm_group(sl, rsl, cc * 128)
                        act(dst[:, c, :], pb[:, :], AF.Tanh, [rpb], [rdst[c]], scale=0.5)
            if mix_stop <= 6:
                return
            for hd in range(4):
                if first_tile:
                    for dc in range(2):
                        S.op("dve", lambda e, i=2 * hd + dc: e.memset(stf[:, i, :], 0.0), [], [rstf[2 * hd + dc]])
                        S.op("dve", lambda e, i=2 * hd + dc: e.memset(stb[:, i, :], 0.0), [], [rstb[2 * hd + dc]])
                gam_c = GAMMA[hd] ** 128
                for n in range(4):
                    cs = slice(n * 128, (n + 1) * 128)
                    sbk, rsbk = bank()
                    mm(sbk[:, 0:128], [(kf[:, 2 * hd + dc, cs], qf[:, 2 * hd + dc, cs]) for dc in range(2)],
                       [rk[2 * hd], rk[2 * hd + 1], rq[2 * hd], rq[2 * hd + 1]], [rsbk])
                    si = nxt("Ssb", 2)
                    dve_tt(Ssb[:, si, :], sbk[:, 0:128], maskT[:, hd * 128:(hd + 1) * 128], ALU.mult, [rsbk, rmask], [rSsb[si]])
                    o1, ro1 = bank()
                    mm(o1[:, :], [(Ssb[:, si, :], vT[:, n, hd * 512:(hd + 1) * 512])], [rSsb[si], rvT[n][hd]], [ro1])
                    o2, ro2 = bank()
                    mm(o2[:, :], [(qf[:, 2 * hd + dc, cs], stb[:, 2 * hd + dc, :]) for dc in range(2)],
                       [rq[2 * hd], rq[2 * hd + 1], rstb[2 * hd], rstb[2 * hd + 1]], [ro2])
                    of, rof = get_tmpf()
                    act(of, o1[:, :], AF.Identity, [ro1], [rof])
                    dve_stt(of, o2[:, :], vcol(V_XI, hd), of, ALU.mult, ALU.add, [ro2, rvecs, rof], [rof])
                    bi = nxt("bn", 2)
                    S.op("dve", lambda e, bi=bi, of=of: e.bn_stats(out=bnst[:, bi, :], in_=of), [rof], [rbn[bi]])
                    rb = [rbn[bi]]
                    m_a, M_a, m_b, M_b = (bnst[:, bi, 1:2], bnst[:, bi, 2:3], bnst[:, bi, 4:5], bnst[:, bi, 5:6])
                    dd_ = bna[:, bi, :]
                    dve_tt(dd_, m_a, m_b, ALU.subtract, rb, rb)
                    dve_stt(bnmv[:, bi, 0:1], dd_, -0.5, m_a, ALU.mult, ALU.add, rb, rb)
                    dve_tt(bnv[:, bi, :], M_a, M_b, ALU.add, rb, rb)
                    dve_ts(bnv[:, bi, :], bnv[:, bi, :], 1.0 / 512.0, EPS, ALU.mult, ALU.add, rb, rb)
                    dve_stt(dd_, dd_, 0.25, dd_, ALU.mult, ALU.mult, rb, rb)
                    dve_tt(bnv[:, bi, :], bnv[:, bi, :], dd_, ALU.add, rb, rb)
                    rsqrt_newton(bnr[:, bi, :], rbn[bi], bnv[:, bi, :], rbn[bi], bna[:, bi, :], rbn[bi])
                    dve_ts(onrm[:, n, :], of, bnmv[:, bi, 0:1], bnr[:, bi, :], ALU.subtract, ALU.mult, [rof, rbn[bi]], [ronrm[n]])
                    for dc in range(2):
                        i = 2 * hd + dc
                        ub, rub = bank()
                        mm(ub[:, :], [(kT[:, n, i * 128:(i + 1) * 128], vT[:, n, hd * 512:(hd + 1) * 512])],
                           [rkT[i], rvT[n][hd]], [rub])
                        dve_stt(stf[:, i, :], stf[:, i, :], gam_c, ub[:, :], ALU.mult, ALU.add, [rstf[i], rub], [rstf[i]])
                        act(stb[:, i, :], stf[:, i, :], AF.Identity, [rstf[i]], [rstb[i]])
                for d4 in range(4):
                    j = hd * 4 + d4
                    tb_o, rtb_o = bank()
                    transposes([(tb_o[:, n * 128:(n + 1) * 128], onrm[:, n, d4 * 128:(d4 + 1) * 128]) for n in range(4)],
                               list(ronrm), [rtb_o])
                    tb, rtb = get_tmpb()
                    act(tb, tb_o[:, :], AF.Identity, [rtb_o, rvecs], [rtb], bias=vcol(V_RGB, j), scale=vcol(V_RGG, j))
                    dve_tt(hid[:, j, :], hid[:, j, :], tb, ALU.mult, [rhid[j], rtb], [rhid[j]])
            if mix_stop <= 7:
                return
            s1b, rs1b = bank()
            s2b, rs2b = bank()
            order = [3, 4, 5, 6, 7, 0, 1, 2]
            for idx, c in enumerate(order):
                first, last = (idx == 0), (idx == 7)
                i = nxt("sq", 4)
                if c >= 3:
                    cb, rcb = bank()
                    for tap in range(CW):
                        di = nxt("dg", 4)
                        act(dg[:, di, :], ident[:, :], AF.Identity, [rident, rvecs], [rdg[di]], scale=vcol(V_WDW, c * CW + tap))
                        mm(cb[:, :], [(dg[:, di, :], u[:, c, tap:tap + T])], [rdg[di], ru[c]], [rcb],
                           start=(tap == 0), stop=(tap == CW - 1))
                    act(kf[:, c, :], cb[:, :], AF.Identity, [rcb, rvecs], [rk[c]], bias=vcol(V_BDW, c))
                    act(sqb[:, i, :], cb[:, :], AF.Square, [rcb, rvecs], [rsq[i]], bias=vcol(V_BDW, c))
                else:
                    ai = nxt("accf", 2)
                    acc = accf[:, ai, :]
                    dve_ts(acc, u[:, c, 0:T], vcol(V_WDW, c * CW), vcol(V_BDW, c), ALU.mult, ALU.add, [ru[c], rvecs], [raccf[ai]])
                    for tap in range(1, CW):
                        dve_stt(acc, u[:, c, tap:tap + T], vcol(V_WDW, c * CW + tap), acc, ALU.mult, ALU.add,
                                [ru[c], rvecs, raccf[ai]], [raccf[ai]])
                    act(kf[:, c, :], acc, AF.Identity, [raccf[ai]], [rk[c]])
                    act(sqb[:, i, :], acc, AF.Square, [raccf[ai]], [rsq[i]])
                act(u[:, c, 0:30], u[:, c, T:T + 30], AF.Identity, [ru[c]], [ru[c]])
                mm(s1b[:, :], [(ones[:, :], kf[:, c, :])], [rk[c], rones], [rs1b], start=first, stop=last)
                mm(s2b[:, :], [(ones[:, :], sqb[:, i, :])], [rsq[i], rones], [rs2b], start=first, stop=last)
            jm = nxt("rstd", 2)
            mean, rmean = rstd[:, jm, :], rrstd[jm]
            dve_ts(mean, s1b[:, :], 1.0 / D, None, ALU.mult, None, [rs1b], [rmean])
            msq, rmsq = get_tmpf()
            dve_tt(msq, mean, mean, ALU.mult, [rmean], [rmsq])
            dve_stt(msq, s2b[:, :], 1.0 / D, msq, ALU.mult, ALU.subtract, [rs2b, rmsq], [rmsq])
            j = nxt("rstd", 2)
            lr, rlr = rstd[:, j, :], rrstd[j]
            dve_ts(msq, msq, EPS, None, ALU.add, None, [rmsq], [rmsq])
            aa, raa = get_tmpf()
            rsqrt_newton(lr, rlr, msq, rmsq, aa, raa)
            dve_tt(mean, mean, lr, ALU.mult, [rmean, rlr], [rmean])
            for c in range(8):
                tf, rtf = get_tmpf()
                dve_tt(tf, kf[:, c, :], lr, ALU.mult, [rk[c], rlr], [rtf])
                dve_tt(tf, tf, mean, ALU.subtract, [rtf, rmean], [rtf])
                act(qf[:, c, :], tf, AF.Silu, [rtf, rvecs], [rq[c]], bias=vcol(V_LNB, c), scale=vcol(V_LNG, c))
            if mix_stop <= 8:
                return
            for i in range(4):
                sl, rsl = next_piece(wro[i, :, :], 4096)
                for e_ in range(2):
                    m = 2 * i + e_
                    pb, rpb = bank()
                    mm(pb[:, :], [(sl[:, j * 256 + e_ * 128: j * 256 + e_ * 128 + 128], hid[:, j, :]) for j in range(16)],
                       [rsl] + rhid[0:16], [rpb])
                    kreg = rkT[(m % 2) * 4:(m % 2) * 4 + 4]
                    dve_stt(t1[:, m, :], gr[:, m, :], 1.0, pb[:, :], ALU.add, ALU.mult, [rpb, rgr[m]], kreg)
            for i in range(2):
                sl, rsl = next_piece(wco[i, :, :], 4096)
                for e_ in range(4):
                    m = 4 * i + e_
                    pb, rpb = bank()
                    mm(pb[:, :], [(sl[:, c * 512 + e_ * 128: c * 512 + e_ * 128 + 128], qf[:, c, :]) for c in range(8)],
                       [rsl] + rq, [rpb])
                    tf, rtf = get_tmpf()
                    act(tf, pb[:, :], AF.Identity, [rpb, rvecs], [rtf], bias=vcol(V_BCO, m))
                    dve_stt(tf, gc[:, m, :], 1.0, tf, ALU.add, ALU.mult, [rtf, rgc[m]], [rtf])
                    kreg = rkT[(m % 2) * 4:(m % 2) * 4 + 4]
                    dve_tt(xn[:, m, :], tf, t1[:, m, :], ALU.add, [rtf] + kreg, [rxn[m]])
            G = drvG(s, 1)
            for i in range(2):
                sl, rsl = next_piece(wout[i, :, :], 4096)
                for e_ in range(4):
                    m = 4 * i + e_
                    pb, rpb = bank()
                    mm(pb[:, :], [(sl[:, c * 512 + e_ * 128: c * 512 + e_ * 128 + 128], xn[:, c, :]) for c in range(8)],
                       [rsl] + rxn, [rpb])
                    dve_stt(h[:, m, :], pb[:, :], G[:, m:m + 1], h[:, m, :], ALU.mult, ALU.add, [rpb, rdrv, rh[m]], [rh[m]])

        for s in range(n_seq):
            for n in range(n_tile):
                tok0 = s * SEQ + n * T
                dma("sp", h[:, :, :], xT[:, :, tok0:tok0 + T], [], rh, "hload")
                dma("sp", posi[:, :], posrep[:, tok0:tok0 + T], [], [rposi], "posld")
                pf, rpf = get_tmpf()
                dve_copy(pf, posi[:, :], [rposi], [rpf])
                dve_ts(pf, pf, vcol(V_FREQ), None, ALU.mult, None, [rpf, rvecs], [rpf])
                kf_, rkf_ = get_tmpf()
                dve_ts(kf_, pf, 1.0 / (2.0 * math.pi), 0.5, ALU.mult, ALU.add, [rpf], [rkf_])
                dve_copy(posi[:, :], kf_, [rkf_], [rposi])
                dve_copy(kf_, posi[:, :], [rposi], [rkf_])
                dve_stt(pf, kf_, -RC1, pf, ALU.mult, ALU.add, [rkf_, rpf], [rpf])
                dve_stt(pf, kf_, -RC2, pf, ALU.mult, ALU.add, [rkf_, rpf], [rpf])
                dve_ts(kf_, pf, -math.pi, 2.0 * math.pi, ALU.is_lt, ALU.mult, [rpf], [rkf_])
                dve_tt(pf, pf, kf_, ALU.add, [rpf, rkf_], [rpf])
                ts_, rts = get_tmpf()
                dve_ts(ts_, pf, -PI_LO, PI_LO, ALU.max, ALU.min, [rpf], [rts])
                act(sinT[:, :], ts_, AF.Sin, [rts], [rsin])
                dve_ts(pf, ts_, 0.5 * math.pi, None, ALU.add, None, [rts], [rpf])
                dve_ts(kf_, pf, math.pi, -2.0 * math.pi, ALU.is_gt, ALU.mult, [rpf], [rkf_])
                dve_tt(pf, pf, kf_, ALU.add, [rpf, rkf_], [rpf])
                tc2, rtc2 = get_tmpf()
                dve_ts(tc2, pf, -PI_LO, PI_LO, ALU.max, ALU.min, [rpf], [rtc2])
                act(cosT[:, :], tc2, AF.Sin, [rtc2], [rcos])

                stages = ["load", "ffn1", "mixer", "ffn2", "final"]
                lvl = stages.index(stop_after)
                if lvl >= 1:
                    norm_mod(s, 0)
                    ffn(s, 0, 0)
                if lvl >= 2:
                    mixer(s, n == 0)
                import os
                dump = os.environ.get("DBG_DUMP", "")
                if dump:
                    srcs = {"go0": (hid, 0, rhid[0:8]), "go1": (hid, 8, rhid[8:16]), "qf": (qf, 0, rq), "kf": (kf, 0, rk),
                            "xn": (xn, 0, rxn), "gr": (gr, 0, rgr), "gc": (gc, 0, rgc), "t1": (t1, 0, [rkT[(m % 2) * 4] for m in range(8)]),
                            "stf": (stf, 0, rstf), "u": (u, 0, ru), "vT": (vT.rearrange("p a (b c) -> p (a b) c", c=512), 0, [rvT[0][0]] * 8),
                            "kT": (kT2[:, :].rearrange("p (a b) -> p a b", a=8), 0, rkT)}
                    src, off, rr = srcs[dump]
                    for c in range(8):
                        sap = src[:, off + c, 0:T] if dump != "u" else src[:, c, 30:30 + T]
                        dve_copy(h[:, c, :], sap, [rr[c]], [rh[c]])
                if lvl >= 3:
                    norm_mod(s, 2)
                    ffn(s, 2, 1)
                if lvl >= 4:
                    rs_ap, rrs = sumsq_rstd()
                    for c in range(8):
                        tf, rtf = get_tmpf()
                        dve_tt(tf, h[:, c, :], rs_ap, ALU.mult, [rh[c], rrs], [rtf])
                        act(h[:, c, :], tf, AF.Identity, [rtf, rg32], [rh[c]], scale=g32[:, 24 + c:25 + c])
                dma("sp", outT[:, :, tok0:tok0 + T], h[:, :, :], rh, [], "hstore")
        S.final_wait("sp", "hstore")

        keys = set(S.cnt.keys()) | set(Sched.ENGS)
        sems = {k: es.enter_context(nc.semaphore("s_" + k)) for k in sorted(keys)}
        with nc.Block() as block:
            @block.tensor
            def _(e):
                S.emit("pe", e, sems)

            @block.scalar
            def _(e):
                S.emit("act", e, sems)

            @block.vector
            def _(e):
                S.emit("dve", e, sems)

            @block.gpsimd
            def _(e):
                S.emit("pool", e, sems)

            @block.sync
            def _(e):
                S.emit("sp", e, sems)
    return nc


def _pieces(W, col_lists):
    d_in = W.shape[0]
    kc = d_in // 128
    out = []
    for cols in col_lists:
        blk = W[:, cols]
        ncol = blk.shape[1]
        out.append(blk.reshape(kc, 128, ncol).transpose(1, 0, 2).reshape(128, kc * ncol))
    return np.ascontiguousarray(np.stack(out)).astype(np.float32, copy=False)


def _fm(v):
    v = np.asarray(v, np.float32).reshape(-1)
    return v.reshape(-1, 128).T


def _gu_cols():
    lists = []
    for p in range(11):
        cols = []
        for jj in range(2):
            j = 2 * p + jj
            cols += list(range(j * 128, (j + 1) * 128)) + list(range(DFF + j * 128, DFF + (j + 1) * 128))
        lists.append(np.array(cols))
    return lists


def _win_cols():
    Q, K, V, GR, GA, GB, TR, TC = 0, 1024, 2048, 4096, 6144, 7168, 8192, 9216
    lists = []
    for base, npc in ((Q, 2), (K, 2), (V, 4), (GR, 4)):
        for i in range(npc):
            lists.append(np.arange(base + i * 512, base + (i + 1) * 512))
    for i in range(4):
        lists.append(np.concatenate([np.arange(GA + 2 * i * 128, GA + (2 * i + 2) * 128),
                                     np.arange(GB + 2 * i * 128, GB + (2 * i + 2) * 128)]))
    for base in (TR, TC):
        for i in range(2):
            lists.append(np.arange(base + i * 512, base + (i + 1) * 512))
    return lists


def _blocks(n, w):
    return [np.arange(i * w, (i + 1) * w) for i in range(n)]


def kernel(x, c, positions, w_mod, b_mod, g_norm1, w_ffn1_gu, w_ffn1_d, g_norm2, w_in, ret_gn_g, ret_gn_b,
           w_ret_o, w_dw, b_dw, conv_ln_g, conv_ln_b, w_conv_o, b_conv_o, w_out, g_norm3, w_ffn2_gu, w_ffn2_d,
           g_normf):
    f32 = np.float32
    x = np.asarray(x, f32)
    c = np.asarray(c, f32)
    positions = np.asarray(positions, np.int32)
    shared = {
        "wmod": _pieces(np.asarray(w_mod, f32)[0], _blocks(18, 512)),
        "wgu1": _pieces(np.asarray(w_ffn1_gu, f32)[0], _gu_cols()),
        "wgu2": _pieces(np.asarray(w_ffn2_gu, f32)[0], _gu_cols()),
        "wd1": _pieces(np.asarray(w_ffn1_d, f32)[0], _blocks(8, 128)),
        "wd2": _pieces(np.asarray(w_ffn2_d, f32)[0], _blocks(8, 128)),
        "win": _pieces(np.asarray(w_in, f32)[0], _win_cols()),
        "wro": _pieces(np.asarray(w_ret_o, f32)[0], _blocks(4, 256)),
        "wco": _pieces(np.asarray(w_conv_o, f32)[0], _blocks(2, 512)),
        "wout": _pieces(np.asarray(w_out, f32)[0], _blocks(2, 512)),
    }
    vecs = np.zeros((128, NV), f32)
    vecs[:, V_GN + 0:V_GN + 8] = _fm(g_norm1)
    vecs[:, V_GN + 8:V_GN + 16] = _fm(g_norm2)
    vecs[:, V_GN + 16:V_GN + 24] = _fm(g_norm3)
    vecs[:, V_GN + 24:V_GN + 32] = _fm(g_normf)
    vecs[:, V_BMOD:V_BMOD + 72] = _fm(b_mod)
    vecs[:, V_RGG:V_RGG + 16] = _fm(ret_gn_g)
    vecs[:, V_RGB:V_RGB + 16] = _fm(ret_gn_b)
    vecs[:, V_BDW:V_BDW + 8] = _fm(b_dw)
    vecs[:, V_LNG:V_LNG + 8] = _fm(conv_ln_g)
    vecs[:, V_LNB:V_LNB + 8] = _fm(conv_ln_b)
    vecs[:, V_BCO:V_BCO + 8] = _fm(b_conv_o)
    wdw = np.asarray(w_dw, f32).reshape(CW, D)
    vecs[:, V_WDW:V_WDW + 8 * CW] = wdw.reshape(CW, 8, 128).transpose(2, 1, 0).reshape(128, 8 * CW)
    half = 128
    vecs[:, V_FREQ] = np.power(f32(10000.0), -np.arange(half, dtype=f32) * f32(2.0 / 256.0)).astype(f32)
    idx = np.arange(128, dtype=np.float64)
    maskT = np.zeros((128, NHEAD, 128), f32)
    for hd in range(NHEAD):
        lg = math.log1p(-2.0 ** (-5.0 - hd))
        vecs[:, V_ZETA + hd] = (np.exp(lg * (127.0 - idx)) / 16.0).astype(f32)
        vecs[:, V_XI + hd] = np.exp(lg * (idx + 1.0)).astype(f32)
        diff = idx[None, :] - idx[:, None]
        maskT[:, hd, :] = np.where(diff >= 0, np.exp(lg * np.maximum(diff, 0.0)) / 16.0, 0.0).astype(f32)
    shared["vecs"] = vecs
    shared["maskT"] = np.ascontiguousarray(maskT.reshape(128, NHEAD * 128))
    shared["ident"] = np.eye(128, dtype=f32)

    in_maps = []
    for i in range(NCORE):
        xs = x[SPC * i:SPC * (i + 1)].reshape(TOK, D)
        xTi = np.ascontiguousarray(xs.T.reshape(8, 128, TOK).transpose(1, 0, 2))
        ps = positions[SPC * i:SPC * (i + 1)].reshape(1, TOK)
        cs = c[SPC * i:SPC * (i + 1)]
        cTi = np.ascontiguousarray(cs.T.reshape(8, 128, SPC).transpose(1, 0, 2).reshape(128, 8 * SPC))
        m = dict(shared)
        m["xT"] = xTi
        m["posrep"] = np.ascontiguousarray(np.broadcast_to(ps, (128, TOK))).astype(np.int32)
        m["cT"] = cTi
        in_maps.append(m)

    nc = build_program()
    res = run_bass_kernel_spmd(nc, in_maps, core_ids=list(range(NCORE)))
    out = np.empty((NCORE * SPC, SEQ, D), f32)
    for i in range(NCORE):
        o = np.asarray(res.results[i]["outT"], f32)
        out[SPC * i:SPC * (i + 1)] = o.transpose(2, 1, 0).reshape(TOK, D).reshape(SPC, SEQ, D)
    return out
```

```python
import math
from contextlib import ExitStack

import numpy as np
import concourse.bass as bass
import concourse.mybir as mybir
from concourse.bass_utils import run_bass_kernel_spmd

F32 = mybir.dt.float32
BF16 = mybir.dt.bfloat16
I32 = mybir.dt.int32
AF = mybir.ActivationFunctionType
ALU = mybir.AluOpType

NCORE = 8
D = 1024
SEQ = 2048
T = 512
NTILE = SEQ // T
SPC = 2
TOK = SPC * SEQ
DFF = 2816
HC = DFF // 128
EPS = 1e-6
NS = 4
SLOT_E = 4096
NHEAD = 4
PST_N = 2
SAME_ENG_WINDOW = 3
CW = 31

V_GN = 0
V_BMOD = 32
V_RGG = 104
V_RGB = 120
V_BDW = 136
V_LNG = 144
V_LNB = 152
V_BCO = 160
V_WDW = 168
V_FREQ = 416
V_ZETA = 417
V_XI = 421
NV = 425

GAMMA = [1.0 - 2.0 ** (-5.0 - h) for h in range(NHEAD)]
RC1 = 6.28125
RC2 = 2.0 * math.pi - RC1
PI_LO = 3.1415925


class Reg:
    __slots__ = ("w", "rs")

    def __init__(self):
        self.w = None
        self.rs = {}


def regs(n):
    return [Reg() for _ in range(n)]


class Sched:
    ENGS = ("pe", "act", "dve", "pool", "sp")

    def __init__(self):
        self.q = {e: [] for e in self.ENGS}
        self.cnt = {}
        self.waited = {e: {} for e in self.ENGS}

    def op(self, eng, fn, reads=(), writes=(), dma_sem=None):
        deps = {}

        def add(k, v):
            if deps.get(k, 0) < v:
                deps[k] = v

        for r in reads:
            if r.w is not None:
                add(*r.w)
        for w in writes:
            if w.w is not None:
                if not (w.w[0] == eng and dma_sem is None):
                    add(*w.w)
            for k, v in w.rs.items():
                if k == eng and dma_sem is None:
                    continue
                add(k, v)
        waits = []
        for k, v in deps.items():
            if k == eng and (eng == "pe" or v <= self.cnt.get(eng, 0) - SAME_ENG_WINDOW):
                continue
            if self.waited[eng].get(k, 0) < v:
                self.waited[eng][k] = v
                waits.append((k, v))
        key = eng if dma_sem is None else dma_sem
        inc = 1 if dma_sem is None else 16
        self.cnt[key] = self.cnt.get(key, 0) + inc
        val = self.cnt[key]
        self.q[eng].append((waits, fn, key, inc))
        for r in reads:
            if r.rs.get(key, 0) < val:
                r.rs[key] = val
        for w in writes:
            w.w = (key, val)
            w.rs = {}
        return (key, val)

    def final_wait(self, eng, key):
        self.q[eng].append(([(key, self.cnt[key])], None, None, 0))

    def emit(self, eng, e, sems):
        for waits, fn, key, inc in self.q[eng]:
            for k, v in waits:
                e.wait_ge(sems[k], v)
            if fn is not None:
                ins = fn(e)
                ins.then_inc(sems[key], inc)


def build_program(n_seq=SPC, n_tile=NTILE, stop_after="final", do_mod=True, mix_stop=99):
    nc = bass.Bass("TRN2", target_bir_lowering=False)
    S = Sched()

    def din(name, shape, dt=F32):
        return nc.dram_tensor(name, shape, dt, kind="ExternalInput").ap()

    xT = din("xT", [128, 8, TOK])
    posrep = din("posrep", [128, TOK], I32)
    cT = din("cT", [128, 16])
    vecs_d = din("vecs", [128, NV])
    mask_d = din("maskT", [128, NHEAD * 128])
    ident_d = din("ident", [128, 128])
    wmod = din("wmod", [18, 128, 4096])
    wgu = [din("wgu1", [11, 128, 4096]), din("wgu2", [11, 128, 4096])]
    wd = [din("wd1", [8, 128, 2816]), din("wd2", [8, 128, 2816])]
    win = din("win", [20, 128, 4096])
    wro = din("wro", [4, 128, 4096])
    wco = din("wco", [2, 128, 4096])
    wout = din("wout", [2, 128, 4096])
    outT = nc.dram_tensor("outT", [128, 8, TOK], F32, kind="ExternalOutput").ap()
    wsc = nc.dram_tensor("wsc", [66, 128, SLOT_E], BF16, kind="Internal").ap()
    rwsc = regs(66)

    with ExitStack() as es:
        def sb(name, shape, dt):
            return es.enter_context(nc.sbuf_tensor(name, shape, dt))

        def pst(name, shape, dt):
            return es.enter_context(nc.psum_tensor(name, shape, dt))

        h = sb("h", [128, 8, T], F32)
        xn = sb("xn", [128, 8, T], BF16)
        hid = sb("hid", [128, HC, T], BF16)
        slots = [sb(f"slot{i}", [128, SLOT_E], BF16) for i in range(NS)]
        qf = sb("qf", [128, 8, T], BF16)
        kf = sb("kf", [128, 8, T], BF16)
        kT2 = sb("kT", [128, 4 * 1024], BF16)
        kT = kT2[:, :].rearrange("p (a b) -> p a b", a=4)
        t1 = kT2[:, :].rearrange("p (a b) -> p a b", a=8)
        vT = sb("vT", [128, 4, 2048], BF16)
        u = sb("u", [128, 8, 30 + T], BF16)
        gr = sb("gr", [128, 8, T], BF16)
        gc = sb("gc", [128, 8, T], BF16)
        stf = sb("stf", [128, 8, 512], F32)
        stb = sb("stb", [128, 8, 512], BF16)
        cosT = sb("cosT", [128, T], F32)
        sinT = sb("sinT", [128, T], F32)
        sqb = sb("sqb", [128, 4, T], BF16)
        tmpf = sb("tmpf", [128, 4, T], F32)
        tmpb = sb("tmpb", [128, 4, T], BF16)
        rstd = sb("rstd", [128, 2, T], F32)
        onrm = sb("onrm", [128, 8, 512], BF16)
        Ssb = sb("Ssb", [128, 2, 128], BF16)
        accf = sb("accf", [128, 1, T], F32)
        posi = accf[:, 0, :].bitcast(I32)
        vecs = sb("vecs_s", [128, NV], F32)
        maskT = sb("mask_s", [128, NHEAD * 128], F32)
        ident = sb("ident_s", [128, 128], BF16)
        ones = sb("ones_s", [128, 128], BF16)
        cTs = sb("cTs", [128, 16], F32)
        cact = sb("cact", [128, 16], BF16)
        modT = sb("modT", [128, 2 * 72], F32)
        g32 = sb("g32", [128, 32], F32)
        drv = sb("drv", [128, 2 * 3 * 2 * 8], F32)
        bnst = sb("bnst", [128, 2, 6], F32)
        bnmv = sb("bnmv", [128, 2, 2], F32)
        dg = sb("dg", [128, 4, 128], BF16)
        bnr = sb("bnr", [128, 2, 1], F32)
        bnv = sb("bnv", [128, 2, 1], F32)
        bna = sb("bna", [128, 2, 1], F32)

        NB = 7
        banks = [pst(f"pb{i}", [128, 512], F32) for i in range(NB)]

        rh, rxn, rhid = regs(8), regs(8), regs(HC)
        rslot = regs(NS)
        rq, rk, rkT = regs(8), regs(8), regs(8)
        rvT = [regs(4) for _ in range(4)]
        ru, rgr, rgc = regs(8), regs(8), regs(8)
        rstf, rstb = regs(8), regs(8)
        rcos, rsin = Reg(), Reg()
        rsq, rtmpf, rtmpb = regs(4), regs(4), regs(4)
        rrstd, ronrm, rSsb, raccf = regs(2), regs(8), regs(2), regs(1)
        rposi = raccf[0]
        rbank, rpsT = regs(NB), regs(2)
        rvecs, rmask, rident, rones, rcT, rcact, rmod, rg32, rdrv = (Reg() for _ in range(9))
        rbn = regs(2)
        rdg = regs(4)
        rlnmean = Reg()

        rot = {"bank": 0, "sq": 0, "tmpf": 0, "tmpb": 0, "rstd": 0, "Ssb": 0, "accf": 0, "psT": 0,
               "slot": 0, "bn": 0, "dg": 0}

        def nxt(name, n):
            i = rot[name]
            rot[name] = (i + 1) % n
            return i

        def bank():
            i = nxt("bank", NB)
            return banks[i], rbank[i]

        def get_tmpf():
            i = nxt("tmpf", 4)
            return tmpf[:, i, :], rtmpf[i]

        def get_tmpb():
            i = nxt("tmpb", 4)
            return tmpb[:, i, :], rtmpb[i]

        def mm(out_ap, pairs, reads, writes, start=True, stop=True):
            def fn(e, out_ap=out_ap, pairs=pairs, start=start, stop=stop):
                n = len(pairs)
                ins = None
                for i, (l, r) in enumerate(pairs):
                    ins = e.matmul(out_ap, lhsT=l, rhs=r, start=(start and i == 0), stop=(stop and i == n - 1))
                return ins
            S.op("pe", fn, reads, writes)

        def transposes(items, reads, writes):
            def fn(e, items=items):
                ins = None
                for o, i_ in items:
                    ins = e.matmul(o, lhsT=i_, rhs=ident[:, :], start=True, stop=True)
                return ins
            S.op("pe", fn, reads + [rident], writes)

        def act(out, in_, func, reads, writes, bias=None, scale=None):
            def fn(e, out=out, in_=in_, func=func, bias=bias, scale=scale):
                kw = {}
                if bias is not None:
                    kw["bias"] = bias
                if scale is not None:
                    kw["scale"] = scale
                return e.activation(out=out, in_=in_, func=func, **kw)
            S.op("act", fn, reads, writes)

        def dve_tt(out, in0, in1, op, reads, writes):
            S.op("dve", lambda e, out=out, in0=in0, in1=in1, op=op: e.tensor_tensor(out=out, in0=in0, in1=in1, op=op),
                 reads, writes)

        def dve_ts(out, in0, s1, s2, op0, op1, reads, writes):
            def fn(e, out=out, in0=in0, s1=s1, s2=s2, op0=op0, op1=op1):
                if op1 is None:
                    return e.tensor_scalar(out=out, in0=in0, scalar1=s1, scalar2=None, op0=op0)
                return e.tensor_scalar(out=out, in0=in0, scalar1=s1, scalar2=s2, op0=op0, op1=op1)
            S.op("dve", fn, reads, writes)

        def dve_stt(out, in0, scalar, in1, op0, op1, reads, writes):
            S.op("dve", lambda e, out=out, in0=in0, scalar=scalar, in1=in1, op0=op0, op1=op1:
                 e.scalar_tensor_tensor(out=out, in0=in0, scalar=scalar, in1=in1, op0=op0, op1=op1), reads, writes)

        def dve_copy(out, in_, reads, writes):
            S.op("dve", lambda e, out=out, in_=in_: e.tensor_copy(out=out, in_=in_), reads, writes)

        def dma(eng, out, in_, reads, writes, semkey):
            S.op(eng, lambda e, out=out, in_=in_: e.dma_start(out=out, in_=in_), reads, writes, dma_sem=semkey)

        wstate = {"pidx": 0, "first": True}

        def next_piece(dram_piece, E, stream=True):
            i = nxt("slot", NS)
            if not stream:
                dma("pool", slots[i][:, 0:E], dram_piece, [], [rslot[i]], f"slot{i}")
                return slots[i], rslot[i]
            p = wstate["pidx"]
            wstate["pidx"] += 1
            if wstate["first"]:
                dma("pool", slots[i][:, 0:E], dram_piece, [], [rslot[i]], f"slot{i}")
                dma("sp", wsc[p, :, 0:E], slots[i][:, 0:E], [rslot[i]], [rwsc[p]], f"wst{i}")
            else:
                dma("pool", slots[i][:, 0:E], wsc[p, :, 0:E], [rwsc[p]], [rslot[i]], f"slot{i}")
            return slots[i], rslot[i]

        def vcol(base, i=0, n=1):
            return vecs[:, base + i: base + i + n]

        dma("sp", vecs[:, :], vecs_d[:, :], [], [rvecs], "const")
        dma("sp", maskT[:, :], mask_d[:, :], [], [rmask], "const")
        dma("sp", cTs[:, :], cT[:, :], [], [rcT], "const")
        dma("pool", ident[:, :], ident_d[:, :], [], [rident], "identld")
        S.op("dve", lambda e: e.memset(ones[:, :], 1.0), [], [rones])
        act(cact[:, :], cTs[:, :], AF.Silu, [rcT], [rcact])
        dve_ts(g32[:, :], vecs[:, V_GN:V_GN + 32], 1.0, None, ALU.mult, None, [rvecs], [rg32])
        dve_ts(vecs[:, V_WDW:V_WDW + 8 * CW], vecs[:, V_WDW:V_WDW + 8 * CW], 0.5, None, ALU.mult, None, [rvecs], [rvecs])
        for p in range(18 if do_mod else 0):
            sl, rsl = next_piece(wmod[p, :, :], 4096, stream=False)
            for cc in range(4):
                ch = 4 * p + cc
                pb, rpb = bank()
                mm(pb[:, 0:2], [(sl[:, kc * 512 + cc * 128: kc * 512 + cc * 128 + 128], cact[:, kc * 2: kc * 2 + 2])
                                for kc in range(8)], [rsl, rcact], [rpb])
                for s in range(2):
                    dve_ts(modT[:, s * 72 + ch: s * 72 + ch + 1], pb[:, s:s + 1], vcol(V_BMOD, ch), None, ALU.add, None,
                           [rpb, rvecs], [rmod])

        def drvA(s, i):
            o = ((s * 3 + i) * 2 + 0) * 8
            return drv[:, o:o + 8]

        def drvG(s, i):
            o = ((s * 3 + i) * 2 + 1) * 8
            return drv[:, o:o + 8]

        def modB(s, i):
            o = s * 72 + (3 * i) * 8
            return modT[:, o:o + 8]

        for s in range(2):
            for i in range(3):
                sc = modT[:, s * 72 + (3 * i + 1) * 8: s * 72 + (3 * i + 1) * 8 + 8]
                gt = modT[:, s * 72 + (3 * i + 2) * 8: s * 72 + (3 * i + 2) * 8 + 8]
                dve_stt(drvA(s, i), sc, 1.0, g32[:, 8 * i: 8 * i + 8], ALU.add, ALU.mult, [rmod, rg32], [rdrv])
                dve_ts(drvG(s, i), gt, 0.5, None, ALU.mult, None, [rmod], [rdrv])

        MAGIC = 0x5f3759df

        def rsqrt_newton(y, ry, v, rv, a, ra):
            yi = y.bitcast(I32)
            dve_ts(yi, v.bitcast(I32), 1, None, ALU.arith_shift_right, None, [rv], [ry])
            dve_ts(yi, yi, -1, MAGIC, ALU.mult, ALU.add, [ry], [ry])
            for _ in range(2):
                dve_stt(a, y, -0.5, y, ALU.mult, ALU.mult, [ry], [ra])
                dve_tt(a, a, v, ALU.mult, [ra, rv], [ra])
                dve_stt(y, a, 1.5, y, ALU.add, ALU.mult, [ra, ry], [ry])

        def sumsq_rstd():
            pb, rpb = bank()
            for c in range(8):
                i = nxt("sq", 4)
                act(sqb[:, i, :], h[:, c, :], AF.Square, [rh[c]], [rsq[i]])
                mm(pb[:, :], [(ones[:, :], sqb[:, i, :])], [rsq[i], rones], [rpb], start=(c == 0), stop=(c == 7))
            j = nxt("rstd", 2)
            vv, rvv = get_tmpf()
            aa, raa = get_tmpf()
            dve_ts(vv, pb[:, :], 1.0 / D, EPS, ALU.mult, ALU.add, [rpb], [rvv])
            rsqrt_newton(rstd[:, j, :], rrstd[j], vv, rvv, aa, raa)
            return rstd[:, j, :], rrstd[j]

        def norm_mod(s, i):
            rs_ap, rrs = sumsq_rstd()
            A = drvA(s, i)
            Bv = modB(s, i)
            for c in range(8):
                tf, rtf = get_tmpf()
                dve_tt(tf, h[:, c, :], rs_ap, ALU.mult, [rh[c], rrs], [rtf])
                act(xn[:, c, :], tf, AF.Identity, [rtf, rdrv, rmod], [rxn[c]], bias=Bv[:, c:c + 1], scale=A[:, c:c + 1])

        def ffn(s, i, which):
            G = drvG(s, i)
            for p in range(11):
                sl, rsl = next_piece(wgu[which][p, :, :], 4096)
                for jj in range(2):
                    j = 2 * p + jj
                    gb, rgb = bank()
                    ub, rub = bank()
                    mm(gb[:, :], [(sl[:, kc * 512 + jj * 256: kc * 512 + jj * 256 + 128], xn[:, kc, :]) for kc in range(8)],
                       [rsl] + rxn, [rgb])
                    mm(ub[:, :], [(sl[:, kc * 512 + jj * 256 + 128: kc * 512 + jj * 256 + 256], xn[:, kc, :]) for kc in range(8)],
                       [rsl] + rxn, [rub])
                    tb, rtb = get_tmpb()
                    act(tb, gb[:, :], AF.Silu, [rgb], [rtb])
                    dve_tt(hid[:, j, :], ub[:, :], tb, ALU.mult, [rub, rtb], [rhid[j]])
            for m in range(8):
                sl, rsl = next_piece(wd[which][m, :, :], 2816)
                yb, ryb = bank()
                mm(yb[:, :], [(sl[:, kc * 128: kc * 128 + 128], hid[:, kc, :]) for kc in range(HC)], [rsl] + rhid, [ryb])
                dve_stt(h[:, m, :], yb[:, :], G[:, m:m + 1], h[:, m, :], ALU.mult, ALU.add, [ryb, rdrv, rh[m]], [rh[m]])

        def rope(x1, r1, x2, r2, o1, ro1, o2, ro2):
            ta, rta = get_tmpf()
            tb_, rtb_ = get_tmpf()
            dve_tt(ta, x1, cosT[:, :], ALU.mult, [r1, rcos], [rta])
            dve_tt(tb_, x2, sinT[:, :], ALU.mult, [r2, rsin], [rtb_])
            dve_tt(o1, ta, tb_, ALU.subtract, [rta, rtb_], [ro1])
            tc_, rtc_ = get_tmpf()
            td, rtd = get_tmpf()
            dve_tt(tc_, x1, sinT[:, :], ALU.mult, [r1, rsin], [rtc_])
            dve_tt(td, x2, cosT[:, :], ALU.mult, [r2, rcos], [rtd])
            dve_tt(o2, tc_, td, ALU.add, [rtc_, rtd], [ro2])

        def fm_group(sl, rsl, off):
            pb, rpb = bank()
            mm(pb[:, :], [(sl[:, kc * 512 + off: kc * 512 + off + 128], xn[:, kc, :]) for kc in range(8)], [rsl] + rxn, [rpb])
            return pb, rpb

        def mixer(s, first_tile):
            norm_mod(s, 1)
            pi = 0
            for which, dst, rdst in ((0, qf, rq), (1, kf, rk)):
                for pp in range(2):
                    sl, rsl = next_piece(win[pi, :, :], 4096)
                    pi += 1
                    for hh in range(2):
                        hd = 2 * pp + hh
                        p1, rp1 = fm_group(sl, rsl, (2 * hh) * 128)
                        p2, rp2 = fm_group(sl, rsl, (2 * hh + 1) * 128)
                        rope(p1[:, :], rp1, p2[:, :], rp2, dst[:, 2 * hd, :], rdst[2 * hd], dst[:, 2 * hd + 1, :], rdst[2 * hd + 1])
            if mix_stop <= 1:
                return
            for c in range(8):
                hd = c // 2
                tb_k, rtb_k = bank()
                transposes([(tb_k[:, n * 128:(n + 1) * 128], kf[:, c, n * 128:(n + 1) * 128]) for n in range(4)], [rk[c]], [rtb_k])
                act(kT[:, :, c * 128:(c + 1) * 128], tb_k[:, :].rearrange("p (a b) -> p a b", a=4), AF.Identity, [rtb_k, rvecs],
                    [rkT[c]], scale=vcol(V_ZETA, hd))
            if mix_stop <= 2:
                return
            for hd in range(4):
                sl, rsl = next_piece(win[pi, :, :], 4096)
                pi += 1
                for n in range(4):
                    pb, rpb = bank()
                    import os
                    if os.environ.get("DBG_V", "both") in ("both", "mm"):
                        mm(pb[:, :], [(xn[:, kc, n * 128:(n + 1) * 128], sl[:, kc * 512: kc * 512 + 512]) for kc in range(8)],
                           [rsl] + rxn, [rpb])
                    if os.environ.get("DBG_V", "both") in ("both", "act"):
                        act(vT[:, n, hd * 512:(hd + 1) * 512], pb[:, :], AF.Identity, [rpb], [rvT[n][hd]])
            if mix_stop <= 3:
                return
            for pp in range(4):
                sl, rsl = next_piece(win[pi, :, :], 4096)
                pi += 1
                for cc in range(4):
                    j = 4 * pp + cc
                    pb, rpb = fm_group(sl, rsl, cc * 128)
                    act(hid[:, j, :], pb[:, :], AF.Silu, [rpb], [rhid[j]])
            if mix_stop <= 4:
                return
            if first_tile:
                for c in range(8):
                    S.op("dve", lambda e, c=c: e.memset(u[:, c, 0:30], 0.0), [], [ru[c]])
            for pp in range(4):
                sl, rsl = next_piece(win[pi, :, :], 4096)
                pi += 1
                for e_ in range(2):
                    c = 2 * pp + e_
                    pa, rpa = fm_group(sl, rsl, e_ * 128)
                    pb, rpb = fm_group(sl, rsl, (2 + e_) * 128)
                    tb, rtb = get_tmpb()
                    act(tb, pb[:, :], AF.Tanh, [rpb], [rtb], scale=0.5)
                    dve_stt(u[:, c, 30:30 + T], tb, 1.0, pa[:, :], ALU.add, ALU.mult, [rpa, rtb], [ru[c]])
            if mix_stop <= 5:
                return
            for dst, rdst in ((gr, rgr), (gc, rgc)):
                for pp in range(2):
                    sl, rsl = next_piece(win[pi, :, :], 4096)
                    pi += 1
                    for cc in range(4):
                        c = 4 * pp + cc
                        pb, rpb = fm_group(sl, rsl, cc * 128)
                        act(dst[:, c, :], pb[:, :], AF.Tanh, [rpb], [rdst[c]], scale=0.5)
            if mix_stop <= 6:
                return
            for hp in range(2):
                heads = (2 * hp, 2 * hp + 1)
                if first_tile:
                    for hd in heads:
                        for dc in range(2):
                            S.op("dve", lambda e, i=2 * hd + dc: e.memset(stf[:, i, :], 0.0), [], [rstf[2 * hd + dc]])
                            S.op("dve", lambda e, i=2 * hd + dc: e.memset(stb[:, i, :], 0.0), [], [rstb[2 * hd + dc]])
                for n in range(4):
                    cs = slice(n * 128, (n + 1) * 128)
                    st_ = {}
                    for hd in heads:
                        sbk, rsbk = bank()
                        mm(sbk[:, 0:128], [(kf[:, 2 * hd + dc, cs], qf[:, 2 * hd + dc, cs]) for dc in range(2)],
                           [rk[2 * hd], rk[2 * hd + 1], rq[2 * hd], rq[2 * hd + 1]], [rsbk])
                        st_[hd] = {"sbk": sbk, "rsbk": rsbk}
                    for hd in heads:
                        d_ = st_[hd]
                        si = nxt("Ssb", 2)
                        dve_tt(Ssb[:, si, :], d_["sbk"][:, 0:128], maskT[:, hd * 128:(hd + 1) * 128], ALU.mult,
                               [d_["rsbk"], rmask], [rSsb[si]])
                        d_["si"] = si
                    for hd in heads:
                        d_ = st_[hd]
                        si = d_["si"]
                        o1, ro1 = bank()
                        mm(o1[:, :], [(Ssb[:, si, :], vT[:, n, hd * 512:(hd + 1) * 512])], [rSsb[si], rvT[n][hd]], [ro1])
                        o2, ro2 = bank()
                        mm(o2[:, :], [(qf[:, 2 * hd + dc, cs], stb[:, 2 * hd + dc, :]) for dc in range(2)],
                           [rq[2 * hd], rq[2 * hd + 1], rstb[2 * hd], rstb[2 * hd + 1]], [ro2])
                        d_.update(o1=o1, ro1=ro1, o2=o2, ro2=ro2)
                    for hd in heads:
                        d_ = st_[hd]
                        of, rof = get_tmpf()
                        act(of, d_["o1"][:, :], AF.Identity, [d_["ro1"]], [rof])
                        d_.update(of=of, rof=rof)
                    for hd in heads:
                        d_ = st_[hd]
                        of, rof = d_["of"], d_["rof"]
                        dve_stt(of, d_["o2"][:, :], vcol(V_XI, hd), of, ALU.mult, ALU.add, [d_["ro2"], rvecs, rof], [rof])
                        bi = hd % 2
                        S.op("dve", lambda e, bi=bi, of=of: e.bn_stats(out=bnst[:, bi, :], in_=of), [rof], [rbn[bi]])
                        rb = [rbn[bi]]
                        m_a, M_a, m_b, M_b = (bnst[:, bi, 1:2], bnst[:, bi, 2:3], bnst[:, bi, 4:5], bnst[:, bi, 5:6])
                        dd_ = bna[:, bi, :]
                        dve_tt(dd_, m_a, m_b, ALU.subtract, rb, rb)
                        dve_stt(bnmv[:, bi, 0:1], dd_, -0.5, m_a, ALU.mult, ALU.add, rb, rb)
                        dve_tt(bnv[:, bi, :], M_a, M_b, ALU.add, rb, rb)
                        dve_ts(bnv[:, bi, :], bnv[:, bi, :], 1.0 / 512.0, EPS, ALU.mult, ALU.add, rb, rb)
                        dve_stt(dd_, dd_, 0.25, dd_, ALU.mult, ALU.mult, rb, rb)
                        dve_tt(bnv[:, bi, :], bnv[:, bi, :], dd_, ALU.add, rb, rb)
                        rsqrt_newton(bnr[:, bi, :], rbn[bi], bnv[:, bi, :], rbn[bi], bna[:, bi, :], rbn[bi])
                        oi = (hd % 2) * 4 + n
                        dve_ts(onrm[:, oi, :], of, bnmv[:, bi, 0:1], bnr[:, bi, :], ALU.subtract, ALU.mult, [rof, rbn[bi]], [ronrm[oi]])
                    for hd in heads:
                        d_ = st_[hd]
                        d_["ub"] = []
                        for dc in range(2):
                            i = 2 * hd + dc
                            ub, rub = bank()
                            mm(ub[:, :], [(kT[:, n, i * 128:(i + 1) * 128], vT[:, n, hd * 512:(hd + 1) * 512])],
                               [rkT[i], rvT[n][hd]], [rub])
                            d_["ub"].append((ub, rub))
                    for hd in heads:
                        gam_c = GAMMA[hd] ** 128
                        for dc in range(2):
                            i = 2 * hd + dc
                            ub, rub = st_[hd]["ub"][dc]
                            dve_stt(stf[:, i, :], stf[:, i, :], gam_c, ub[:, :], ALU.mult, ALU.add, [rstf[i], rub], [rstf[i]])
                            act(stb[:, i, :], stf[:, i, :], AF.Identity, [rstf[i]], [rstb[i]])
                for hd in heads:
                    for d4 in range(4):
                        j = hd * 4 + d4
                        tb_o, rtb_o = bank()
                        ob = (hd % 2) * 4
                        transposes([(tb_o[:, n * 128:(n + 1) * 128], onrm[:, ob + n, d4 * 128:(d4 + 1) * 128]) for n in range(4)],
                                   list(ronrm[ob:ob + 4]), [rtb_o])
                        tb, rtb = get_tmpb()
                        act(tb, tb_o[:, :], AF.Identity, [rtb_o, rvecs], [rtb], bias=vcol(V_RGB, j), scale=vcol(V_RGG, j))
                        dve_tt(hid[:, j, :], hid[:, j, :], tb, ALU.mult, [rhid[j], rtb], [rhid[j]])
            if mix_stop <= 7:
                return
            s1b, rs1b = bank()
            s2b, rs2b = bank()
            order = [3, 4, 5, 6, 7, 0, 1, 2]
            for idx, c in enumerate(order):
                first, last = (idx == 0), (idx == 7)
                i = nxt("sq", 4)
                if c >= 3:
                    cb, rcb = bank()
                    for tap in range(CW):
                        di = nxt("dg", 4)
                        act(dg[:, di, :], ident[:, :], AF.Identity, [rident, rvecs], [rdg[di]], scale=vcol(V_WDW, c * CW + tap))
                        mm(cb[:, :], [(dg[:, di, :], u[:, c, tap:tap + T])], [rdg[di], ru[c]], [rcb],
                           start=(tap == 0), stop=(tap == CW - 1))
                    act(kf[:, c, :], cb[:, :], AF.Identity, [rcb, rvecs], [rk[c]], bias=vcol(V_BDW, c))
                    act(sqb[:, i, :], cb[:, :], AF.Square, [rcb, rvecs], [rsq[i]], bias=vcol(V_BDW, c))
                else:
                    ai = nxt("accf", 1)
                    acc = accf[:, ai, :]
                    dve_ts(acc, u[:, c, 0:T], vcol(V_WDW, c * CW), vcol(V_BDW, c), ALU.mult, ALU.add, [ru[c], rvecs], [raccf[ai]])
                    for tap in range(1, CW):
                        dve_stt(acc, u[:, c, tap:tap + T], vcol(V_WDW, c * CW + tap), acc, ALU.mult, ALU.add,
                                [ru[c], rvecs, raccf[ai]], [raccf[ai]])
                    act(kf[:, c, :], acc, AF.Identity, [raccf[ai]], [rk[c]])
                    act(sqb[:, i, :], acc, AF.Square, [raccf[ai]], [rsq[i]])
                act(u[:, c, 0:30], u[:, c, T:T + 30], AF.Identity, [ru[c]], [ru[c]])
                mm(s1b[:, :], [(ones[:, :], kf[:, c, :])], [rk[c], rones], [rs1b], start=first, stop=last)
                mm(s2b[:, :], [(ones[:, :], sqb[:, i, :])], [rsq[i], rones], [rs2b], start=first, stop=last)
            jm = nxt("rstd", 2)
            mean, rmean = rstd[:, jm, :], rrstd[jm]
            dve_ts(mean, s1b[:, :], 1.0 / D, None, ALU.mult, None, [rs1b], [rmean])
            msq, rmsq = get_tmpf()
            dve_tt(msq, mean, mean, ALU.mult, [rmean], [rmsq])
            dve_stt(msq, s2b[:, :], 1.0 / D, msq, ALU.mult, ALU.subtract, [rs2b, rmsq], [rmsq])
            j = nxt("rstd", 2)
            lr, rlr = rstd[:, j, :], rrstd[j]
            dve_ts(msq, msq, EPS, None, ALU.add, None, [rmsq], [rmsq])
            aa, raa = get_tmpf()
            rsqrt_newton(lr, rlr, msq, rmsq, aa, raa)
            dve_tt(mean, mean, lr, ALU.mult, [rmean, rlr], [rmean])
            for c in range(8):
                tf, rtf = get_tmpf()
                dve_tt(tf, kf[:, c, :], lr, ALU.mult, [rk[c], rlr], [rtf])
                dve_tt(tf, tf, mean, ALU.subtract, [rtf, rmean], [rtf])
                act(qf[:, c, :], tf, AF.Silu, [rtf, rvecs], [rq[c]], bias=vcol(V_LNB, c), scale=vcol(V_LNG, c))
            if mix_stop <= 8:
                return
            for i in range(4):
                sl, rsl = next_piece(wro[i, :, :], 4096)
                for e_ in range(2):
                    m = 2 * i + e_
                    pb, rpb = bank()
                    mm(pb[:, :], [(sl[:, j * 256 + e_ * 128: j * 256 + e_ * 128 + 128], hid[:, j, :]) for j in range(16)],
                       [rsl] + rhid[0:16], [rpb])
                    kreg = rkT[(m % 2) * 4:(m % 2) * 4 + 4]
                    dve_stt(t1[:, m, :], gr[:, m, :], 1.0, pb[:, :], ALU.add, ALU.mult, [rpb, rgr[m]], kreg)
            for i in range(2):
                sl, rsl = next_piece(wco[i, :, :], 4096)
                for e_ in range(4):
                    m = 4 * i + e_
                    pb, rpb = bank()
                    mm(pb[:, :], [(sl[:, c * 512 + e_ * 128: c * 512 + e_ * 128 + 128], qf[:, c, :]) for c in range(8)],
                       [rsl] + rq, [rpb])
                    tf, rtf = get_tmpf()
                    act(tf, pb[:, :], AF.Identity, [rpb, rvecs], [rtf], bias=vcol(V_BCO, m))
                    dve_stt(tf, gc[:, m, :], 1.0, tf, ALU.add, ALU.mult, [rtf, rgc[m]], [rtf])
                    kreg = rkT[(m % 2) * 4:(m % 2) * 4 + 4]
                    dve_tt(xn[:, m, :], tf, t1[:, m, :], ALU.add, [rtf] + kreg, [rxn[m]])
            G = drvG(s, 1)
            for i in range(2):
                sl, rsl = next_piece(wout[i, :, :], 4096)
                for e_ in range(4):
                    m = 4 * i + e_
                    pb, rpb = bank()
                    mm(pb[:, :], [(sl[:, c * 512 + e_ * 128: c * 512 + e_ * 128 + 128], xn[:, c, :]) for c in range(8)],
                       [rsl] + rxn, [rpb])
                    dve_stt(h[:, m, :], pb[:, :], G[:, m:m + 1], h[:, m, :], ALU.mult, ALU.add, [rpb, rdrv, rh[m]], [rh[m]])

        for s in range(n_seq):
            for n in range(n_tile):
                tok0 = s * SEQ + n * T
                wstate["pidx"] = 0
                wstate["first"] = (s == 0 and n == 0)
                dma("sp", h[:, :, :], xT[:, :, tok0:tok0 + T], [], rh, "hload")
                dma("sp", posi[:, :], posrep[:, tok0:tok0 + T], [], [rposi], "posld")
                pf, rpf = get_tmpf()
                dve_copy(pf, posi[:, :], [rposi], [rpf])
                dve_ts(pf, pf, vcol(V_FREQ), None, ALU.mult, None, [rpf, rvecs], [rpf])
                kf_, rkf_ = get_tmpf()
                dve_ts(kf_, pf, 1.0 / (2.0 * math.pi), 0.5, ALU.mult, ALU.add, [rpf], [rkf_])
                dve_copy(posi[:, :], kf_, [rkf_], [rposi])
                dve_copy(kf_, posi[:, :], [rposi], [rkf_])
                dve_stt(pf, kf_, -RC1, pf, ALU.mult, ALU.add, [rkf_, rpf], [rpf])
                dve_stt(pf, kf_, -RC2, pf, ALU.mult, ALU.add, [rkf_, rpf], [rpf])
                dve_ts(kf_, pf, -math.pi, 2.0 * math.pi, ALU.is_lt, ALU.mult, [rpf], [rkf_])
                dve_tt(pf, pf, kf_, ALU.add, [rpf, rkf_], [rpf])
                ts_, rts = get_tmpf()
                dve_ts(ts_, pf, -PI_LO, PI_LO, ALU.max, ALU.min, [rpf], [rts])
                act(sinT[:, :], ts_, AF.Sin, [rts], [rsin])
                dve_ts(pf, ts_, 0.5 * math.pi, None, ALU.add, None, [rts], [rpf])
                dve_ts(kf_, pf, math.pi, -2.0 * math.pi, ALU.is_gt, ALU.mult, [rpf], [rkf_])
                dve_tt(pf, pf, kf_, ALU.add, [rpf, rkf_], [rpf])
                tc2, rtc2 = get_tmpf()
                dve_ts(tc2, pf, -PI_LO, PI_LO, ALU.max, ALU.min, [rpf], [rtc2])
                act(cosT[:, :], tc2, AF.Sin, [rtc2], [rcos])

                stages = ["load", "ffn1", "mixer", "ffn2", "final"]
                lvl = stages.index(stop_after)
                if lvl >= 1:
                    norm_mod(s, 0)
                    ffn(s, 0, 0)
                if lvl >= 2:
                    mixer(s, n == 0)
                import os
                dump = os.environ.get("DBG_DUMP", "")
                if dump:
                    srcs = {"go0": (hid, 0, rhid[0:8]), "go1": (hid, 8, rhid[8:16]), "qf": (qf, 0, rq), "kf": (kf, 0, rk),
                            "xn": (xn, 0, rxn), "gr": (gr, 0, rgr), "gc": (gc, 0, rgc), "t1": (t1, 0, [rkT[(m % 2) * 4] for m in range(8)]),
                            "stf": (stf, 0, rstf), "u": (u, 0, ru), "vT": (vT.rearrange("p a (b c) -> p (a b) c", c=512), 0, [rvT[0][0]] * 8),
                            "kT": (kT2[:, :].rearrange("p (a b) -> p a b", a=8), 0, rkT)}
                    src, off, rr = srcs[dump]
                    for c in range(8):
                        sap = src[:, off + c, 0:T] if dump != "u" else src[:, c, 30:30 + T]
                        dve_copy(h[:, c, :], sap, [rr[c]], [rh[c]])
                if lvl >= 3:
                    norm_mod(s, 2)
                    ffn(s, 2, 1)
                if lvl >= 4:
                    rs_ap, rrs = sumsq_rstd()
                    for c in range(8):
                        tf, rtf = get_tmpf()
                        dve_tt(tf, h[:, c, :], rs_ap, ALU.mult, [rh[c], rrs], [rtf])
                        act(h[:, c, :], tf, AF.Identity, [rtf, rg32], [rh[c]], scale=g32[:, 24 + c:25 + c])
                dma("sp", outT[:, :, tok0:tok0 + T], h[:, :, :], rh, [], "hstore")
        S.final_wait("sp", "hstore")

        keys = set(S.cnt.keys()) | set(Sched.ENGS)
        sems = {k: es.enter_context(nc.semaphore("s_" + k)) for k in sorted(keys)}
        with nc.Block() as block:
            @block.tensor
            def _(e):
                S.emit("pe", e, sems)

            @block.scalar
            def _(e):
                S.emit("act", e, sems)

            @block.vector
            def _(e):
                S.emit("dve", e, sems)

            @block.gpsimd
            def _(e):
                S.emit("pool", e, sems)

            @block.sync
            def _(e):
                S.emit("sp", e, sems)
    return nc


def _pieces(W, col_lists):
    d_in = W.shape[0]
    kc = d_in // 128
    out = []
    for cols in col_lists:
        blk = W[:, cols]
        ncol = blk.shape[1]
        out.append(blk.reshape(kc, 128, ncol).transpose(1, 0, 2).reshape(128, kc * ncol))
    return np.ascontiguousarray(np.stack(out)).astype(np.float32, copy=False)


def _fm(v):
    v = np.asarray(v, np.float32).reshape(-1)
    return v.reshape(-1, 128).T


def _gu_cols():
    lists = []
    for p in range(11):
        cols = []
        for jj in range(2):
            j = 2 * p + jj
            cols += list(range(j * 128, (j + 1) * 128)) + list(range(DFF + j * 128, DFF + (j + 1) * 128))
        lists.append(np.array(cols))
    return lists


def _win_cols():
    Q, K, V, GR, GA, GB, TR, TC = 0, 1024, 2048, 4096, 6144, 7168, 8192, 9216
    lists = []
    for base, npc in ((Q, 2), (K, 2), (V, 4), (GR, 4)):
        for i in range(npc):
            lists.append(np.arange(base + i * 512, base + (i + 1) * 512))
    for i in range(4):
        lists.append(np.concatenate([np.arange(GA + 2 * i * 128, GA + (2 * i + 2) * 128),
                                     np.arange(GB + 2 * i * 128, GB + (2 * i + 2) * 128)]))
    for base in (TR, TC):
        for i in range(2):
            lists.append(np.arange(base + i * 512, base + (i + 1) * 512))
    return lists


def _blocks(n, w):
    return [np.arange(i * w, (i + 1) * w) for i in range(n)]


def kernel(x, c, positions, w_mod, b_mod, g_norm1, w_ffn1_gu, w_ffn1_d, g_norm2, w_in, ret_gn_g, ret_gn_b,
           w_ret_o, w_dw, b_dw, conv_ln_g, conv_ln_b, w_conv_o, b_conv_o, w_out, g_norm3, w_ffn2_gu, w_ffn2_d,
           g_normf):
    f32 = np.float32
    x = np.asarray(x, f32)
    c = np.asarray(c, f32)
    positions = np.asarray(positions, np.int32)
    shared = {
        "wmod": _pieces(np.asarray(w_mod, f32)[0], _blocks(18, 512)),
        "wgu1": _pieces(np.asarray(w_ffn1_gu, f32)[0], _gu_cols()),
        "wgu2": _pieces(np.asarray(w_ffn2_gu, f32)[0], _gu_cols()),
        "wd1": _pieces(np.asarray(w_ffn1_d, f32)[0], _blocks(8, 128)),
        "wd2": _pieces(np.asarray(w_ffn2_d, f32)[0], _blocks(8, 128)),
        "win": _pieces(np.asarray(w_in, f32)[0], _win_cols()),
        "wro": _pieces(np.asarray(w_ret_o, f32)[0], _blocks(4, 256)),
        "wco": _pieces(np.asarray(w_conv_o, f32)[0], _blocks(2, 512)),
        "wout": _pieces(np.asarray(w_out, f32)[0], _blocks(2, 512)),
    }
    vecs = np.zeros((128, NV), f32)
    vecs[:, V_GN + 0:V_GN + 8] = _fm(g_norm1)
    vecs[:, V_GN + 8:V_GN + 16] = _fm(g_norm2)
    vecs[:, V_GN + 16:V_GN + 24] = _fm(g_norm3)
    vecs[:, V_GN + 24:V_GN + 32] = _fm(g_normf)
    vecs[:, V_BMOD:V_BMOD + 72] = _fm(b_mod)
    vecs[:, V_RGG:V_RGG + 16] = _fm(ret_gn_g)
    vecs[:, V_RGB:V_RGB + 16] = _fm(ret_gn_b)
    vecs[:, V_BDW:V_BDW + 8] = _fm(b_dw)
    vecs[:, V_LNG:V_LNG + 8] = _fm(conv_ln_g)
    vecs[:, V_LNB:V_LNB + 8] = _fm(conv_ln_b)
    vecs[:, V_BCO:V_BCO + 8] = _fm(b_conv_o)
    wdw = np.asarray(w_dw, f32).reshape(CW, D)
    vecs[:, V_WDW:V_WDW + 8 * CW] = wdw.reshape(CW, 8, 128).transpose(2, 1, 0).reshape(128, 8 * CW)
    half = 128
    vecs[:, V_FREQ] = np.power(f32(10000.0), -np.arange(half, dtype=f32) * f32(2.0 / 256.0)).astype(f32)
    idx = np.arange(128, dtype=np.float64)
    maskT = np.zeros((128, NHEAD, 128), f32)
    for hd in range(NHEAD):
        lg = math.log1p(-2.0 ** (-5.0 - hd))
        vecs[:, V_ZETA + hd] = (np.exp(lg * (127.0 - idx)) / 16.0).astype(f32)
        vecs[:, V_XI + hd] = np.exp(lg * (idx + 1.0)).astype(f32)
        diff = idx[None, :] - idx[:, None]
        maskT[:, hd, :] = np.where(diff >= 0, np.exp(lg * np.maximum(diff, 0.0)) / 16.0, 0.0).astype(f32)
    shared["vecs"] = vecs
    shared["maskT"] = np.ascontiguousarray(maskT.reshape(128, NHEAD * 128))
    shared["ident"] = np.eye(128, dtype=f32)

    in_maps = []
    for i in range(NCORE):
        xs = x[SPC * i:SPC * (i + 1)].reshape(TOK, D)
        xTi = np.ascontiguousarray(xs.T.reshape(8, 128, TOK).transpose(1, 0, 2))
        ps = positions[SPC * i:SPC * (i + 1)].reshape(1, TOK)
        cs = c[SPC * i:SPC * (i + 1)]
        cTi = np.ascontiguousarray(cs.T.reshape(8, 128, SPC).transpose(1, 0, 2).reshape(128, 8 * SPC))
        m = dict(shared)
        m["xT"] = xTi
        m["posrep"] = np.ascontiguousarray(np.broadcast_to(ps, (128, TOK))).astype(np.int32)
        m["cT"] = cTi
        in_maps.append(m)

    nc = build_program()
    res = run_bass_kernel_spmd(nc, in_maps, core_ids=list(range(NCORE)))
    out = np.empty((NCORE * SPC, SEQ, D), f32)
    for i in range(NCORE):
        o = np.asarray(res.results[i]["outT"], f32)
        out[SPC * i:SPC * (i + 1)] = o.transpose(2, 1, 0).reshape(TOK, D).reshape(SPC, SEQ, D)
    return out
```

```python
import math
from contextlib import ExitStack

import numpy as np
import concourse.bass as bass
import concourse.mybir as mybir
from concourse.bass_utils import run_bass_kernel_spmd

F32 = mybir.dt.float32
BF16 = mybir.dt.bfloat16
I32 = mybir.dt.int32
AF = mybir.ActivationFunctionType
ALU = mybir.AluOpType

NCORE = 8
D = 1024
SEQ = 2048
T = 512
NTILE = SEQ // T
SPC = 2
TOK = SPC * SEQ
DFF = 2816
HC = DFF // 128
EPS = 1e-6
NS = 4
SLOT_E = 4096
NHEAD = 4
PST_N = 2
SAME_ENG_WINDOW = 3
CW = 31

V_GN = 0
V_BMOD = 32
V_RGG = 104
V_RGB = 120
V_BDW = 136
V_LNG = 144
V_LNB = 152
V_BCO = 160
V_WDW = 168
V_FREQ = 416
V_ZETA = 417
V_XI = 421
NV = 425

GAMMA = [1.0 - 2.0 ** (-5.0 - h) for h in range(NHEAD)]
RC1 = 6.28125
RC2 = 2.0 * math.pi - RC1
PI_LO = 3.1415925


class Reg:
    __slots__ = ("w", "rs", "ww")

    def __init__(self):
        self.w = None
        self.rs = {}
        self.ww = False


def regs(n):
    return [Reg() for _ in range(n)]


class Sched:
    ENGS = ("pe", "act", "dve", "pool", "sp")

    def __init__(self):
        self.q = {e: [] for e in self.ENGS}
        self.cnt = {}
        self.waited = {e: {} for e in self.ENGS}

    def op(self, eng, fn, reads=(), writes=(), dma_sem=None, wide=False):
        deps = {}

        def add(k, v):
            if deps.get(k, 0) < v:
                deps[k] = v

        for r in reads:
            if r.w is not None:
                if r.w[0] == eng and eng == "dve" and r.ww:
                    continue
                add(*r.w)
        for w in writes:
            if w.w is not None:
                if not (w.w[0] == eng and dma_sem is None):
                    add(*w.w)
            for k, v in w.rs.items():
                if k == eng and dma_sem is None:
                    continue
                add(k, v)
        waits = []
        for k, v in deps.items():
            if k == eng and (eng == "pe" or v <= self.cnt.get(eng, 0) - SAME_ENG_WINDOW):
                continue
            if self.waited[eng].get(k, 0) < v:
                self.waited[eng][k] = v
                waits.append((k, v))
        key = eng if dma_sem is None else dma_sem
        inc = 1 if dma_sem is None else 16
        self.cnt[key] = self.cnt.get(key, 0) + inc
        val = self.cnt[key]
        self.q[eng].append((waits, fn, key, inc))
        for r in reads:
            if r.rs.get(key, 0) < val:
                r.rs[key] = val
        for w in writes:
            w.w = (key, val)
            w.rs = {}
            w.ww = bool(wide)
        return (key, val)

    def final_wait(self, eng, key):
        self.q[eng].append(([(key, self.cnt[key])], None, None, 0))

    def emit(self, eng, e, sems):
        for waits, fn, key, inc in self.q[eng]:
            for k, v in waits:
                e.wait_ge(sems[k], v)
            if fn is not None:
                ins = fn(e)
                ins.then_inc(sems[key], inc)


def build_program(n_seq=SPC, n_tile=NTILE, stop_after="final", do_mod=True, mix_stop=99):
    nc = bass.Bass("TRN2", target_bir_lowering=False)
    S = Sched()

    def din(name, shape, dt=F32):
        return nc.dram_tensor(name, shape, dt, kind="ExternalInput").ap()

    xT = din("xT", [128, 8, TOK])
    posrep = din("posrep", [128, TOK], I32)
    cT = din("cT", [128, 16])
    vecs_d = din("vecs", [128, NV])
    mask_d = din("maskT", [128, NHEAD * 128])
    ident_d = din("ident", [128, 128])
    wmod = din("wmod", [18, 128, 4096])
    wgu = [din("wgu1", [11, 128, 4096]), din("wgu2", [11, 128, 4096])]
    wd = [din("wd1", [8, 128, 2816]), din("wd2", [8, 128, 2816])]
    win = din("win", [20, 128, 4096])
    wro = din("wro", [4, 128, 4096])
    wco = din("wco", [2, 128, 4096])
    wout = din("wout", [2, 128, 4096])
    outT = nc.dram_tensor("outT", [128, 8, TOK], F32, kind="ExternalOutput").ap()

    with ExitStack() as es:
        def sb(name, shape, dt):
            return es.enter_context(nc.sbuf_tensor(name, shape, dt))

        def pst(name, shape, dt):
            return es.enter_context(nc.psum_tensor(name, shape, dt))

        h = sb("h", [128, 8, T], F32)
        xn = sb("xn", [128, 8, T], BF16)
        hid = sb("hid", [128, HC, T], BF16)
        slots = [sb(f"slot{i}", [128, SLOT_E], BF16) for i in range(NS)]
        qf = sb("qf", [128, 8, T], BF16)
        kf = sb("kf", [128, 8, T], BF16)
        kT2 = sb("kT", [128, 4 * 1024], BF16)
        kT = kT2[:, :].rearrange("p (a b) -> p a b", a=4)
        t1 = kT2[:, :].rearrange("p (a b) -> p a b", a=8)
        vT = sb("vT", [128, 4, 2048], BF16)
        u = sb("u", [128, 8, 30 + T], BF16)
        gr = sb("gr", [128, 8, T], BF16)
        gc = sb("gc", [128, 8, T], BF16)
        stf = sb("stf", [128, 8, 512], F32)
        stb = sb("stb", [128, 8, 512], BF16)
        cosT = sb("cosT", [128, T], F32)
        sinT = sb("sinT", [128, T], F32)
        posi = sb("posi", [128, T], I32)
        sqb = sb("sqb", [128, 4, T], BF16)
        tmpf = sb("tmpf", [128, 4, T], F32)
        tmpb = sb("tmpb", [128, 4, T], BF16)
        rstd = sb("rstd", [128, 2, T], F32)
        onrm = sb("onrm", [128, 4, 512], BF16)
        Ssb = sb("Ssb", [128, 2, 128], BF16)
        accf = sb("accf", [128, 2, T], F32)
        vecs = sb("vecs_s", [128, NV], F32)
        maskT = sb("mask_s", [128, NHEAD * 128], F32)
        ident = sb("ident_s", [128, 128], BF16)
        ones = sb("ones_s", [128, 128], BF16)
        cTs = sb("cTs", [128, 16], F32)
        cact = sb("cact", [128, 16], BF16)
        modT = sb("modT", [128, 2 * 72], F32)
        g32 = sb("g32", [128, 32], F32)
        drv = sb("drv", [128, 2 * 3 * 2 * 8], F32)
        bnst = sb("bnst", [128, 2, 6], F32)
        bnmv = sb("bnmv", [128, 2, 2], F32)
        dg = sb("dg", [128, 4, 128], BF16)
        bnr = sb("bnr", [128, 2, 1], F32)
        bnv = sb("bnv", [128, 2, 1], F32)
        bna = sb("bna", [128, 2, 1], F32)

        NB = 7
        banks = [pst(f"pb{i}", [128, 512], F32) for i in range(NB)]

        rh, rxn, rhid = regs(8), regs(8), regs(HC)
        rslot = regs(NS)
        rq, rk, rkT = regs(8), regs(8), regs(8)
        rvT = [regs(4) for _ in range(4)]
        ru, rgr, rgc = regs(8), regs(8), regs(8)
        rstf, rstb = regs(8), regs(8)
        rcos, rsin, rposi = Reg(), Reg(), Reg()
        rsq, rtmpf, rtmpb = regs(4), regs(4), regs(4)
        rrstd, ronrm, rSsb, raccf = regs(2), regs(4), regs(2), regs(2)
        rbank, rpsT = regs(NB), regs(2)
        rvecs, rmask, rident, rones, rcT, rcact, rmod, rg32, rdrv = (Reg() for _ in range(9))
        rbn = regs(2)
        rdg = regs(4)
        rlnmean = Reg()

        rot = {"bank": 0, "sq": 0, "tmpf": 0, "tmpb": 0, "rstd": 0, "Ssb": 0, "accf": 0, "psT": 0,
               "slot": 0, "bn": 0, "dg": 0}

        def nxt(name, n):
            i = rot[name]
            rot[name] = (i + 1) % n
            return i

        def bank():
            i = nxt("bank", NB)
            return banks[i], rbank[i]

        def get_tmpf():
            i = nxt("tmpf", 4)
            return tmpf[:, i, :], rtmpf[i]

        def get_tmpb():
            i = nxt("tmpb", 4)
            return tmpb[:, i, :], rtmpb[i]

        def _wide(ap):
            n = 1
            for d in ap.shape[1:]:
                n *= int(d)
            return n >= 256

        def mm(out_ap, pairs, reads, writes, start=True, stop=True):
            def fn(e, out_ap=out_ap, pairs=pairs, start=start, stop=stop):
                n = len(pairs)
                ins = None
                for i, (l, r) in enumerate(pairs):
                    ins = e.matmul(out_ap, lhsT=l, rhs=r, start=(start and i == 0), stop=(stop and i == n - 1))
                return ins
            S.op("pe", fn, reads, writes)

        def transposes(items, reads, writes):
            def fn(e, items=items):
                ins = None
                for o, i_ in items:
                    ins = e.matmul(o, lhsT=i_, rhs=ident[:, :], start=True, stop=True)
                return ins
            S.op("pe", fn, reads + [rident], writes)

        def act(out, in_, func, reads, writes, bias=None, scale=None):
            def fn(e, out=out, in_=in_, func=func, bias=bias, scale=scale):
                kw = {}
                if bias is not None:
                    kw["bias"] = bias
                if scale is not None:
                    kw["scale"] = scale
                return e.activation(out=out, in_=in_, func=func, **kw)
            S.op("act", fn, reads, writes)

        def dve_tt(out, in0, in1, op, reads, writes):
            S.op("dve", lambda e, out=out, in0=in0, in1=in1, op=op: e.tensor_tensor(out=out, in0=in0, in1=in1, op=op),
                 reads, writes, wide=_wide(out))

        def dve_ts(out, in0, s1, s2, op0, op1, reads, writes):
            def fn(e, out=out, in0=in0, s1=s1, s2=s2, op0=op0, op1=op1):
                if op1 is None:
                    return e.tensor_scalar(out=out, in0=in0, scalar1=s1, scalar2=None, op0=op0)
                return e.tensor_scalar(out=out, in0=in0, scalar1=s1, scalar2=s2, op0=op0, op1=op1)
            S.op("dve", fn, reads, writes, wide=_wide(out))

        def dve_stt(out, in0, scalar, in1, op0, op1, reads, writes):
            S.op("dve", lambda e, out=out, in0=in0, scalar=scalar, in1=in1, op0=op0, op1=op1:
                 e.scalar_tensor_tensor(out=out, in0=in0, scalar=scalar, in1=in1, op0=op0, op1=op1), reads, writes,
                 wide=_wide(out))

        def dve_copy(out, in_, reads, writes):
            S.op("dve", lambda e, out=out, in_=in_: e.tensor_copy(out=out, in_=in_), reads, writes, wide=_wide(out))

        def dma(eng, out, in_, reads, writes, semkey):
            S.op(eng, lambda e, out=out, in_=in_: e.dma_start(out=out, in_=in_), reads, writes, dma_sem=semkey)

        def next_piece(dram_piece, E):
            i = nxt("slot", NS)
            dma("pool", slots[i][:, 0:E], dram_piece, [], [rslot[i]], f"slot{i}")
            return slots[i], rslot[i]

        def vcol(base, i=0, n=1):
            return vecs[:, base + i: base + i + n]

        dma("sp", vecs[:, :], vecs_d[:, :], [], [rvecs], "const")
        dma("sp", maskT[:, :], mask_d[:, :], [], [rmask], "const")
        dma("sp", cTs[:, :], cT[:, :], [], [rcT], "const")
        dma("pool", ident[:, :], ident_d[:, :], [], [rident], "identld")
        S.op("dve", lambda e: e.memset(ones[:, :], 1.0), [], [rones])
        act(cact[:, :], cTs[:, :], AF.Silu, [rcT], [rcact])
        dve_ts(g32[:, :], vecs[:, V_GN:V_GN + 32], 1.0, None, ALU.mult, None, [rvecs], [rg32])
        dve_ts(vecs[:, V_WDW:V_WDW + 8 * CW], vecs[:, V_WDW:V_WDW + 8 * CW], 0.5, None, ALU.mult, None, [rvecs], [rvecs])
        for p in range(18 if do_mod else 0):
            sl, rsl = next_piece(wmod[p, :, :], 4096)
            for cc in range(4):
                ch = 4 * p + cc
                pb, rpb = bank()
                mm(pb[:, 0:2], [(sl[:, kc * 512 + cc * 128: kc * 512 + cc * 128 + 128], cact[:, kc * 2: kc * 2 + 2])
                                for kc in range(8)], [rsl, rcact], [rpb])
                for s in range(2):
                    dve_ts(modT[:, s * 72 + ch: s * 72 + ch + 1], pb[:, s:s + 1], vcol(V_BMOD, ch), None, ALU.add, None,
                           [rpb, rvecs], [rmod])

        def drvA(s, i):
            o = ((s * 3 + i) * 2 + 0) * 8
            return drv[:, o:o + 8]

        def drvG(s, i):
            o = ((s * 3 + i) * 2 + 1) * 8
            return drv[:, o:o + 8]

        def modB(s, i):
            o = s * 72 + (3 * i) * 8
            return modT[:, o:o + 8]

        for s in range(2):
            for i in range(3):
                sc = modT[:, s * 72 + (3 * i + 1) * 8: s * 72 + (3 * i + 1) * 8 + 8]
                gt = modT[:, s * 72 + (3 * i + 2) * 8: s * 72 + (3 * i + 2) * 8 + 8]
                dve_stt(drvA(s, i), sc, 1.0, g32[:, 8 * i: 8 * i + 8], ALU.add, ALU.mult, [rmod, rg32], [rdrv])
                dve_ts(drvG(s, i), gt, 0.5, None, ALU.mult, None, [rmod], [rdrv])

        MAGIC = 0x5f3759df

        def rsqrt_newton(y, ry, v, rv, a, ra):
            yi = y.bitcast(I32)
            dve_ts(yi, v.bitcast(I32), 1, None, ALU.arith_shift_right, None, [rv], [ry])
            dve_ts(yi, yi, -1, MAGIC, ALU.mult, ALU.add, [ry], [ry])
            for _ in range(2):
                dve_stt(a, y, -0.5, y, ALU.mult, ALU.mult, [ry], [ra])
                dve_tt(a, a, v, ALU.mult, [ra, rv], [ra])
                dve_stt(y, a, 1.5, y, ALU.add, ALU.mult, [ra, ry], [ry])

        def sumsq_rstd():
            pb, rpb = bank()
            for c in range(8):
                i = nxt("sq", 4)
                act(sqb[:, i, :], h[:, c, :], AF.Square, [rh[c]], [rsq[i]])
                mm(pb[:, :], [(ones[:, :], sqb[:, i, :])], [rsq[i], rones], [rpb], start=(c == 0), stop=(c == 7))
            j = nxt("rstd", 2)
            vv, rvv = get_tmpf()
            aa, raa = get_tmpf()
            dve_ts(vv, pb[:, :], 1.0 / D, EPS, ALU.mult, ALU.add, [rpb], [rvv])
            rsqrt_newton(rstd[:, j, :], rrstd[j], vv, rvv, aa, raa)
            return rstd[:, j, :], rrstd[j]

        def norm_mod(s, i):
            rs_ap, rrs = sumsq_rstd()
            A = drvA(s, i)
            Bv = modB(s, i)
            for c in range(8):
                tf, rtf = get_tmpf()
                dve_tt(tf, h[:, c, :], rs_ap, ALU.mult, [rh[c], rrs], [rtf])
                act(xn[:, c, :], tf, AF.Identity, [rtf, rdrv, rmod], [rxn[c]], bias=Bv[:, c:c + 1], scale=A[:, c:c + 1])

        def ffn(s, i, which):
            G = drvG(s, i)
            for p in range(11):
                sl, rsl = next_piece(wgu[which][p, :, :], 4096)
                for jj in range(2):
                    j = 2 * p + jj
                    gb, rgb = bank()
                    ub, rub = bank()
                    mm(gb[:, :], [(sl[:, kc * 512 + jj * 256: kc * 512 + jj * 256 + 128], xn[:, kc, :]) for kc in range(8)],
                       [rsl] + rxn, [rgb])
                    mm(ub[:, :], [(sl[:, kc * 512 + jj * 256 + 128: kc * 512 + jj * 256 + 256], xn[:, kc, :]) for kc in range(8)],
                       [rsl] + rxn, [rub])
                    tb, rtb = get_tmpb()
                    act(tb, gb[:, :], AF.Silu, [rgb], [rtb])
                    dve_tt(hid[:, j, :], ub[:, :], tb, ALU.mult, [rub, rtb], [rhid[j]])
            for m in range(8):
                sl, rsl = next_piece(wd[which][m, :, :], 2816)
                yb, ryb = bank()
                mm(yb[:, :], [(sl[:, kc * 128: kc * 128 + 128], hid[:, kc, :]) for kc in range(HC)], [rsl] + rhid, [ryb])
                dve_stt(h[:, m, :], yb[:, :], G[:, m:m + 1], h[:, m, :], ALU.mult, ALU.add, [ryb, rdrv, rh[m]], [rh[m]])

        def rope(x1, r1, x2, r2, o1, ro1, o2, ro2):
            ta, rta = get_tmpf()
            tb_, rtb_ = get_tmpf()
            dve_tt(ta, x1, cosT[:, :], ALU.mult, [r1, rcos], [rta])
            dve_tt(tb_, x2, sinT[:, :], ALU.mult, [r2, rsin], [rtb_])
            dve_tt(o1, ta, tb_, ALU.subtract, [rta, rtb_], [ro1])
            tc_, rtc_ = get_tmpf()
            td, rtd = get_tmpf()
            dve_tt(tc_, x1, sinT[:, :], ALU.mult, [r1, rsin], [rtc_])
            dve_tt(td, x2, cosT[:, :], ALU.mult, [r2, rcos], [rtd])
            dve_tt(o2, tc_, td, ALU.add, [rtc_, rtd], [ro2])

        def fm_group(sl, rsl, off):
            pb, rpb = bank()
            mm(pb[:, :], [(sl[:, kc * 512 + off: kc * 512 + off + 128], xn[:, kc, :]) for kc in range(8)], [rsl] + rxn, [rpb])
            return pb, rpb

        def mixer(s, first_tile):
            norm_mod(s, 1)
            pi = 0
            for which, dst, rdst in ((0, qf, rq), (1, kf, rk)):
                for pp in range(2):
                    sl, rsl = next_piece(win[pi, :, :], 4096)
                    pi += 1
                    for hh in range(2):
                        hd = 2 * pp + hh
                        p1, rp1 = fm_group(sl, rsl, (2 * hh) * 128)
                        p2, rp2 = fm_group(sl, rsl, (2 * hh + 1) * 128)
                        rope(p1[:, :], rp1, p2[:, :], rp2, dst[:, 2 * hd, :], rdst[2 * hd], dst[:, 2 * hd + 1, :], rdst[2 * hd + 1])
            if mix_stop <= 1:
                return
            for c in range(8):
                hd = c // 2
                tb_k, rtb_k = bank()
                transposes([(tb_k[:, n * 128:(n + 1) * 128], kf[:, c, n * 128:(n + 1) * 128]) for n in range(4)], [rk[c]], [rtb_k])
                act(kT[:, :, c * 128:(c + 1) * 128], tb_k[:, :].rearrange("p (a b) -> p a b", a=4), AF.Identity, [rtb_k, rvecs],
                    [rkT[c]], scale=vcol(V_ZETA, hd))
            if mix_stop <= 2:
                return
            for hd in range(4):
                sl, rsl = next_piece(win[pi, :, :], 4096)
                pi += 1
                for n in range(4):
                    pb, rpb = bank()
                    import os
                    if os.environ.get("DBG_V", "both") in ("both", "mm"):
                        mm(pb[:, :], [(xn[:, kc, n * 128:(n + 1) * 128], sl[:, kc * 512: kc * 512 + 512]) for kc in range(8)],
                           [rsl] + rxn, [rpb])
                    if os.environ.get("DBG_V", "both") in ("both", "act"):
                        act(vT[:, n, hd * 512:(hd + 1) * 512], pb[:, :], AF.Identity, [rpb], [rvT[n][hd]])
            if mix_stop <= 3:
                return
            for pp in range(4):
                sl, rsl = next_piece(win[pi, :, :], 4096)
                pi += 1
                for cc in range(4):
                    j = 4 * pp + cc
                    pb, rpb = fm_group(sl, rsl, cc * 128)
                    act(hid[:, j, :], pb[:, :], AF.Silu, [rpb], [rhid[j]])
            if mix_stop <= 4:
                return
            if first_tile:
                for c in range(8):
                    S.op("dve", lambda e, c=c: e.memset(u[:, c, 0:30], 0.0), [], [ru[c]])
            for pp in range(4):
                sl, rsl = next_piece(win[pi, :, :], 4096)
                pi += 1
                for e_ in range(2):
                    c = 2 * pp + e_
                    pa, rpa = fm_group(sl, rsl, e_ * 128)
                    pb, rpb = fm_group(sl, rsl, (2 + e_) * 128)
                    tb, rtb = get_tmpb()
                    act(tb, pb[:, :], AF.Tanh, [rpb], [rtb], scale=0.5)
                    dve_stt(u[:, c, 30:30 + T], tb, 1.0, pa[:, :], ALU.add, ALU.mult, [rpa, rtb], [ru[c]])
            if mix_stop <= 5:
                return
            for dst, rdst in ((gr, rgr), (gc, rgc)):
                for pp in range(2):
                    sl, rsl = next_piece(win[pi, :, :], 4096)
                    pi += 1
                    for cc in range(4):
                        c = 4 * pp + cc
                        pb, rpb = fm_group(sl, rsl, cc * 128)
                        act(dst[:, c, :], pb[:, :], AF.Tanh, [rpb], [rdst[c]], scale=0.5)
            if mix_stop <= 6:
                return
            for hd in range(4):
                if first_tile:
                    for dc in range(2):
                        S.op("dve", lambda e, i=2 * hd + dc: e.memset(stf[:, i, :], 0.0), [], [rstf[2 * hd + dc]])
                        S.op("dve", lambda e, i=2 * hd + dc: e.memset(stb[:, i, :], 0.0), [], [rstb[2 * hd + dc]])
                gam_c = GAMMA[hd] ** 128
                for n in range(4):
                    cs = slice(n * 128, (n + 1) * 128)
                    sbk, rsbk = bank()
                    mm(sbk[:, 0:128], [(kf[:, 2 * hd + dc, cs], qf[:, 2 * hd + dc, cs]) for dc in range(2)],
                       [rk[2 * hd], rk[2 * hd + 1], rq[2 * hd], rq[2 * hd + 1]], [rsbk])
                    si = nxt("Ssb", 2)
                    dve_tt(Ssb[:, si, :], sbk[:, 0:128], maskT[:, hd * 128:(hd + 1) * 128], ALU.mult, [rsbk, rmask], [rSsb[si]])
                    o1, ro1 = bank()
                    mm(o1[:, :], [(Ssb[:, si, :], vT[:, n, hd * 512:(hd + 1) * 512])], [rSsb[si], rvT[n][hd]], [ro1])
                    o2, ro2 = bank()
                    mm(o2[:, :], [(qf[:, 2 * hd + dc, cs], stb[:, 2 * hd + dc, :]) for dc in range(2)],
                       [rq[2 * hd], rq[2 * hd + 1], rstb[2 * hd], rstb[2 * hd + 1]], [ro2])
                    of, rof = get_tmpf()
                    act(of, o1[:, :], AF.Identity, [ro1], [rof])
                    dve_stt(of, o2[:, :], vcol(V_XI, hd), of, ALU.mult, ALU.add, [ro2, rvecs, rof], [rof])
                    bi = nxt("bn", 2)
                    S.op("dve", lambda e, bi=bi, of=of: e.bn_stats(out=bnst[:, bi, :], in_=of), [rof], [rbn[bi]])
                    rb = [rbn[bi]]
                    m_a, M_a, m_b, M_b = (bnst[:, bi, 1:2], bnst[:, bi, 2:3], bnst[:, bi, 4:5], bnst[:, bi, 5:6])
                    dd_ = bna[:, bi, :]
                    dve_tt(dd_, m_a, m_b, ALU.subtract, rb, rb)
                    dve_stt(bnmv[:, bi, 0:1], dd_, -0.5, m_a, ALU.mult, ALU.add, rb, rb)
                    dve_tt(bnv[:, bi, :], M_a, M_b, ALU.add, rb, rb)
                    dve_ts(bnv[:, bi, :], bnv[:, bi, :], 1.0 / 512.0, EPS, ALU.mult, ALU.add, rb, rb)
                    dve_stt(dd_, dd_, 0.25, dd_, ALU.mult, ALU.mult, rb, rb)
                    dve_tt(bnv[:, bi, :], bnv[:, bi, :], dd_, ALU.add, rb, rb)
                    rsqrt_newton(bnr[:, bi, :], rbn[bi], bnv[:, bi, :], rbn[bi], bna[:, bi, :], rbn[bi])
                    dve_ts(onrm[:, n, :], of, bnmv[:, bi, 0:1], bnr[:, bi, :], ALU.subtract, ALU.mult, [rof, rbn[bi]], [ronrm[n]])
                    for dc in range(2):
                        i = 2 * hd + dc
                        ub, rub = bank()
                        mm(ub[:, :], [(kT[:, n, i * 128:(i + 1) * 128], vT[:, n, hd * 512:(hd + 1) * 512])],
                           [rkT[i], rvT[n][hd]], [rub])
                        dve_stt(stf[:, i, :], stf[:, i, :], gam_c, ub[:, :], ALU.mult, ALU.add, [rstf[i], rub], [rstf[i]])
                        act(stb[:, i, :], stf[:, i, :], AF.Identity, [rstf[i]], [rstb[i]])
                for d4 in range(4):
                    j = hd * 4 + d4
                    tb_o, rtb_o = bank()
                    transposes([(tb_o[:, n * 128:(n + 1) * 128], onrm[:, n, d4 * 128:(d4 + 1) * 128]) for n in range(4)],
                               list(ronrm), [rtb_o])
                    tb, rtb = get_tmpb()
                    act(tb, tb_o[:, :], AF.Identity, [rtb_o, rvecs], [rtb], bias=vcol(V_RGB, j), scale=vcol(V_RGG, j))
                    dve_tt(hid[:, j, :], hid[:, j, :], tb, ALU.mult, [rhid[j], rtb], [rhid[j]])
            if mix_stop <= 7:
                return
            s1b, rs1b = bank()
            s2b, rs2b = bank()
            order = [3, 4, 5, 6, 7, 0, 1, 2]
            for idx, c in enumerate(order):
                first, last = (idx == 0), (idx == 7)
                i = nxt("sq", 4)
                if c >= 3:
                    cb, rcb = bank()
                    for tap in range(CW):
                        di = nxt("dg", 4)
                        act(dg[:, di, :], ident[:, :], AF.Identity, [rident, rvecs], [rdg[di]], scale=vcol(V_WDW, c * CW + tap))
                        mm(cb[:, :], [(dg[:, di, :], u[:, c, tap:tap + T])], [rdg[di], ru[c]], [rcb],
                           start=(tap == 0), stop=(tap == CW - 1))
                    act(kf[:, c, :], cb[:, :], AF.Identity, [rcb, rvecs], [rk[c]], bias=vcol(V_BDW, c))
                    act(sqb[:, i, :], cb[:, :], AF.Square, [rcb, rvecs], [rsq[i]], bias=vcol(V_BDW, c))
                else:
                    ai = nxt("accf", 2)
                    acc = accf[:, ai, :]
                    dve_ts(acc, u[:, c, 0:T], vcol(V_WDW, c * CW), vcol(V_BDW, c), ALU.mult, ALU.add, [ru[c], rvecs], [raccf[ai]])
                    for tap in range(1, CW):
                        dve_stt(acc, u[:, c, tap:tap + T], vcol(V_WDW, c * CW + tap), acc, ALU.mult, ALU.add,
                                [ru[c], rvecs, raccf[ai]], [raccf[ai]])
                    act(kf[:, c, :], acc, AF.Identity, [raccf[ai]], [rk[c]])
                    act(sqb[:, i, :], acc, AF.Square, [raccf[ai]], [rsq[i]])
                act(u[:, c, 0:30], u[:, c, T:T + 30], AF.Identity, [ru[c]], [ru[c]])
                mm(s1b[:, :], [(ones[:, :], kf[:, c, :])], [rk[c], rones], [rs1b], start=first, stop=last)
                mm(s2b[:, :], [(ones[:, :], sqb[:, i, :])], [rsq[i], rones], [rs2b], start=first, stop=last)
            jm = nxt("rstd", 2)
            mean, rmean = rstd[:, jm, :], rrstd[jm]
            dve_ts(mean, s1b[:, :], 1.0 / D, None, ALU.mult, None, [rs1b], [rmean])
            msq, rmsq = get_tmpf()
            dve_tt(msq, mean, mean, ALU.mult, [rmean], [rmsq])
            dve_stt(msq, s2b[:, :], 1.0 / D, msq, ALU.mult, ALU.subtract, [rs2b, rmsq], [rmsq])
            j = nxt("rstd", 2)
            lr, rlr = rstd[:, j, :], rrstd[j]
            dve_ts(msq, msq, EPS, None, ALU.add, None, [rmsq], [rmsq])
            aa, raa = get_tmpf()
            rsqrt_newton(lr, rlr, msq, rmsq, aa, raa)
            dve_tt(mean, mean, lr, ALU.mult, [rmean, rlr], [rmean])
            for c in range(8):
                tf, rtf = get_tmpf()
                dve_tt(tf, kf[:, c, :], lr, ALU.mult, [rk[c], rlr], [rtf])
                dve_tt(tf, tf, mean, ALU.subtract, [rtf, rmean], [rtf])
                act(qf[:, c, :], tf, AF.Silu, [rtf, rvecs], [rq[c]], bias=vcol(V_LNB, c), scale=vcol(V_LNG, c))
            if mix_stop <= 8:
                return
            for i in range(4):
                sl, rsl = next_piece(wro[i, :, :], 4096)
                for e_ in range(2):
                    m = 2 * i + e_
                    pb, rpb = bank()
                    mm(pb[:, :], [(sl[:, j * 256 + e_ * 128: j * 256 + e_ * 128 + 128], hid[:, j, :]) for j in range(16)],
                       [rsl] + rhid[0:16], [rpb])
                    kreg = rkT[(m % 2) * 4:(m % 2) * 4 + 4]
                    dve_stt(t1[:, m, :], gr[:, m, :], 1.0, pb[:, :], ALU.add, ALU.mult, [rpb, rgr[m]], kreg)
            for i in range(2):
                sl, rsl = next_piece(wco[i, :, :], 4096)
                for e_ in range(4):
                    m = 4 * i + e_
                    pb, rpb = bank()
                    mm(pb[:, :], [(sl[:, c * 512 + e_ * 128: c * 512 + e_ * 128 + 128], qf[:, c, :]) for c in range(8)],
                       [rsl] + rq, [rpb])
                    tf, rtf = get_tmpf()
                    act(tf, pb[:, :], AF.Identity, [rpb, rvecs], [rtf], bias=vcol(V_BCO, m))
                    dve_stt(tf, gc[:, m, :], 1.0, tf, ALU.add, ALU.mult, [rtf, rgc[m]], [rtf])
                    kreg = rkT[(m % 2) * 4:(m % 2) * 4 + 4]
                    dve_tt(xn[:, m, :], tf, t1[:, m, :], ALU.add, [rtf] + kreg, [rxn[m]])
            G = drvG(s, 1)
            for i in range(2):
                sl, rsl = next_piece(wout[i, :, :], 4096)
                for e_ in range(4):
                    m = 4 * i + e_
                    pb, rpb = bank()
                    mm(pb[:, :], [(sl[:, c * 512 + e_ * 128: c * 512 + e_ * 128 + 128], xn[:, c, :]) for c in range(8)],
                       [rsl] + rxn, [rpb])
                    dve_stt(h[:, m, :], pb[:, :], G[:, m:m + 1], h[:, m, :], ALU.mult, ALU.add, [rpb, rdrv, rh[m]], [rh[m]])

        for s in range(n_seq):
            for n in range(n_tile):
                tok0 = s * SEQ + n * T
                dma("sp", h[:, :, :], xT[:, :, tok0:tok0 + T], [], rh, "hload")
                dma("sp", posi[:, :], posrep[:, tok0:tok0 + T], [], [rposi], "posld")
                pf, rpf = get_tmpf()
                dve_copy(pf, posi[:, :], [rposi], [rpf])
                dve_ts(pf, pf, vcol(V_FREQ), None, ALU.mult, None, [rpf, rvecs], [rpf])
                kf_, rkf_ = get_tmpf()
                dve_ts(kf_, pf, 1.0 / (2.0 * math.pi), 0.5, ALU.mult, ALU.add, [rpf], [rkf_])
                dve_copy(posi[:, :], kf_, [rkf_], [rposi])
                dve_copy(kf_, posi[:, :], [rposi], [rkf_])
                dve_stt(pf, kf_, -RC1, pf, ALU.mult, ALU.add, [rkf_, rpf], [rpf])
                dve_stt(pf, kf_, -RC2, pf, ALU.mult, ALU.add, [rkf_, rpf], [rpf])
                dve_ts(kf_, pf, -math.pi, 2.0 * math.pi, ALU.is_lt, ALU.mult, [rpf], [rkf_])
                dve_tt(pf, pf, kf_, ALU.add, [rpf, rkf_], [rpf])
                ts_, rts = get_tmpf()
                dve_ts(ts_, pf, -PI_LO, PI_LO, ALU.max, ALU.min, [rpf], [rts])
                act(sinT[:, :], ts_, AF.Sin, [rts], [rsin])
                dve_ts(pf, ts_, 0.5 * math.pi, None, ALU.add, None, [rts], [rpf])
                dve_ts(kf_, pf, math.pi, -2.0 * math.pi, ALU.is_gt, ALU.mult, [rpf], [rkf_])
                dve_tt(pf, pf, kf_, ALU.add, [rpf, rkf_], [rpf])
                tc2, rtc2 = get_tmpf()
                dve_ts(tc2, pf, -PI_LO, PI_LO, ALU.max, ALU.min, [rpf], [rtc2])
                act(cosT[:, :], tc2, AF.Sin, [rtc2], [rcos])

                stages = ["load", "ffn1", "mixer", "ffn2", "final"]
                lvl = stages.index(stop_after)
                if lvl >= 1:
                    norm_mod(s, 0)
                    ffn(s, 0, 0)
                if lvl >= 2:
                    mixer(s, n == 0)
                import os
                dump = os.environ.get("DBG_DUMP", "")
                if dump:
                    srcs = {"go0": (hid, 0, rhid[0:8]), "go1": (hid, 8, rhid[8:16]), "qf": (qf, 0, rq), "kf": (kf, 0, rk),
                            "xn": (xn, 0, rxn), "gr": (gr, 0, rgr), "gc": (gc, 0, rgc), "t1": (t1, 0, [rkT[(m % 2) * 4] for m in range(8)]),
                            "stf": (stf, 0, rstf), "u": (u, 0, ru), "vT": (vT.rearrange("p a (b c) -> p (a b) c", c=512), 0, [rvT[0][0]] * 8),
                            "kT": (kT2[:, :].rearrange("p (a b) -> p a b", a=8), 0, rkT)}
                    src, off, rr = srcs[dump]
                    for c in range(8):
                        sap = src[:, off + c, 0:T] if dump != "u" else src[:, c, 30:30 + T]
                        dve_copy(h[:, c, :], sap, [rr[c]], [rh[c]])
                if lvl >= 3:
                    norm_mod(s, 2)
                    ffn(s, 2, 1)
                if lvl >= 4:
                    rs_ap, rrs = sumsq_rstd()
                    for c in range(8):
                        tf, rtf = get_tmpf()
                        dve_tt(tf, h[:, c, :], rs_ap, ALU.mult, [rh[c], rrs], [rtf])
                        act(h[:, c, :], tf, AF.Identity, [rtf, rg32], [rh[c]], scale=g32[:, 24 + c:25 + c])
                dma("sp", outT[:, :, tok0:tok0 + T], h[:, :, :], rh, [], "hstore")
        S.final_wait("sp", "hstore")

        keys = set(S.cnt.keys()) | set(Sched.ENGS)
        sems = {k: es.enter_context(nc.semaphore("s_" + k)) for k in sorted(keys)}
        with nc.Block() as block:
            @block.tensor
            def _(e):
                S.emit("pe", e, sems)

            @block.scalar
            def _(e):
                S.emit("act", e, sems)

            @block.vector
            def _(e):
                S.emit("dve", e, sems)

            @block.gpsimd
            def _(e):
                S.emit("pool", e, sems)

            @block.sync
            def _(e):
                S.emit("sp", e, sems)
    return nc


def _pieces(W, col_lists):
    d_in = W.shape[0]
    kc = d_in // 128
    out = []
    for cols in col_lists:
        blk = W[:, cols]
        ncol = blk.shape[1]
        out.append(blk.reshape(kc, 128, ncol).transpose(1, 0, 2).reshape(128, kc * ncol))
    return np.ascontiguousarray(np.stack(out)).astype(np.float32, copy=False)


def _fm(v):
    v = np.asarray(v, np.float32).reshape(-1)
    return v.reshape(-1, 128).T


def _gu_cols():
    lists = []
    for p in range(11):
        cols = []
        for jj in range(2):
            j = 2 * p + jj
            cols += list(range(j * 128, (j + 1) * 128)) + list(range(DFF + j * 128, DFF + (j + 1) * 128))
        lists.append(np.array(cols))
    return lists


def _win_cols():
    Q, K, V, GR, GA, GB, TR, TC = 0, 1024, 2048, 4096, 6144, 7168, 8192, 9216
    lists = []
    for base, npc in ((Q, 2), (K, 2), (V, 4), (GR, 4)):
        for i in range(npc):
            lists.append(np.arange(base + i * 512, base + (i + 1) * 512))
    for i in range(4):
        lists.append(np.concatenate([np.arange(GA + 2 * i * 128, GA + (2 * i + 2) * 128),
                                     np.arange(GB + 2 * i * 128, GB + (2 * i + 2) * 128)]))
    for base in (TR, TC):
        for i in range(2):
            lists.append(np.arange(base + i * 512, base + (i + 1) * 512))
    return lists


def _blocks(n, w):
    return [np.arange(i * w, (i + 1) * w) for i in range(n)]


def kernel(x, c, positions, w_mod, b_mod, g_norm1, w_ffn1_gu, w_ffn1_d, g_norm2, w_in, ret_gn_g, ret_gn_b,
           w_ret_o, w_dw, b_dw, conv_ln_g, conv_ln_b, w_conv_o, b_conv_o, w_out, g_norm3, w_ffn2_gu, w_ffn2_d,
           g_normf):
    f32 = np.float32
    x = np.asarray(x, f32)
    c = np.asarray(c, f32)
    positions = np.asarray(positions, np.int32)
    shared = {
        "wmod": _pieces(np.asarray(w_mod, f32)[0], _blocks(18, 512)),
        "wgu1": _pieces(np.asarray(w_ffn1_gu, f32)[0], _gu_cols()),
        "wgu2": _pieces(np.asarray(w_ffn2_gu, f32)[0], _gu_cols()),
        "wd1": _pieces(np.asarray(w_ffn1_d, f32)[0], _blocks(8, 128)),
        "wd2": _pieces(np.asarray(w_ffn2_d, f32)[0], _blocks(8, 128)),
        "win": _pieces(np.asarray(w_in, f32)[0], _win_cols()),
        "wro": _pieces(np.asarray(w_ret_o, f32)[0], _blocks(4, 256)),
        "wco": _pieces(np.asarray(w_conv_o, f32)[0], _blocks(2, 512)),
        "wout": _pieces(np.asarray(w_out, f32)[0], _blocks(2, 512)),
    }
    vecs = np.zeros((128, NV), f32)
    vecs[:, V_GN + 0:V_GN + 8] = _fm(g_norm1)
    vecs[:, V_GN + 8:V_GN + 16] = _fm(g_norm2)
    vecs[:, V_GN + 16:V_GN + 24] = _fm(g_norm3)
    vecs[:, V_GN + 24:V_GN + 32] = _fm(g_normf)
    vecs[:, V_BMOD:V_BMOD + 72] = _fm(b_mod)
    vecs[:, V_RGG:V_RGG + 16] = _fm(ret_gn_g)
    vecs[:, V_RGB:V_RGB + 16] = _fm(ret_gn_b)
    vecs[:, V_BDW:V_BDW + 8] = _fm(b_dw)
    vecs[:, V_LNG:V_LNG + 8] = _fm(conv_ln_g)
    vecs[:, V_LNB:V_LNB + 8] = _fm(conv_ln_b)
    vecs[:, V_BCO:V_BCO + 8] = _fm(b_conv_o)
    wdw = np.asarray(w_dw, f32).reshape(CW, D)
    vecs[:, V_WDW:V_WDW + 8 * CW] = wdw.reshape(CW, 8, 128).transpose(2, 1, 0).reshape(128, 8 * CW)
    half = 128
    vecs[:, V_FREQ] = np.power(f32(10000.0), -np.arange(half, dtype=f32) * f32(2.0 / 256.0)).astype(f32)
    idx = np.arange(128, dtype=np.float64)
    maskT = np.zeros((128, NHEAD, 128), f32)
    for hd in range(NHEAD):
        lg = math.log1p(-2.0 ** (-5.0 - hd))
        vecs[:, V_ZETA + hd] = (np.exp(lg * (127.0 - idx)) / 16.0).astype(f32)
        vecs[:, V_XI + hd] = np.exp(lg * (idx + 1.0)).astype(f32)
        diff = idx[None, :] - idx[:, None]
        maskT[:, hd, :] = np.where(diff >= 0, np.exp(lg * np.maximum(diff, 0.0)) / 16.0, 0.0).astype(f32)
    shared["vecs"] = vecs
    shared["maskT"] = np.ascontiguousarray(maskT.reshape(128, NHEAD * 128))
    shared["ident"] = np.eye(128, dtype=f32)

    in_maps = []
    for i in range(NCORE):
        xs = x[SPC * i:SPC * (i + 1)].reshape(TOK, D)
        xTi = np.ascontiguousarray(xs.T.reshape(8, 128, TOK).transpose(1, 0, 2))
        ps = positions[SPC * i:SPC * (i + 1)].reshape(1, TOK)
        cs = c[SPC * i:SPC * (i + 1)]
        cTi = np.ascontiguousarray(cs.T.reshape(8, 128, SPC).transpose(1, 0, 2).reshape(128, 8 * SPC))
        m = dict(shared)
        m["xT"] = xTi
        m["posrep"] = np.ascontiguousarray(np.broadcast_to(ps, (128, TOK))).astype(np.int32)
        m["cT"] = cTi
        in_maps.append(m)

    nc = build_program()
    res = run_bass_kernel_spmd(nc, in_maps, core_ids=list(range(NCORE)))
    out = np.empty((NCORE * SPC, SEQ, D), f32)
    for i in range(NCORE):
        o = np.asarray(res.results[i]["outT"], f32)
        out[SPC * i:SPC * (i + 1)] = o.transpose(2, 1, 0).reshape(TOK, D).reshape(SPC, SEQ, D)
    return out
```
